# Optimizing a Trainium2 kernel written in Bass

```python
import jax, jax.numpy as jnp
from jax import lax
import numpy as np

D_MODEL = 2048
BATCH = 16
SEQ = 256
DEPTH = 1
DEC_BATCH = 2
DEC_SEQ = 4096
PAST_LEN = 256

GRID_W = 64
D_MIX = D_MODEL
N_HEADS = 8
KV_HEADS = 2
HEAD_DIM = 128
GQA_GROUP = N_HEADS // KV_HEADS
ATTN_W = N_HEADS * HEAD_DIM
KV_W = KV_HEADS * HEAD_DIM
G_HEADS = 8
G_DIM = (D_MIX - ATTN_W) // G_HEADS
GMLP_W = G_HEADS * G_DIM
IN_W = ATTN_W + 2 * KV_W + 2 * GMLP_W
CHUNK = 128
Q_BLOCK = 128
ROPE_THETA = 10000.0
AXIS_DIM = HEAD_DIM // 2
N_EXPERTS = 32
TOP_K = 4
D_FF = D_MODEL
SWIGLU_ALPHA = 1.702
SWIGLU_LIMIT = 7.0
MOE_BLOCK = 128
N_MOD = 6
EPS = 1e-6

kernel_name = 'hymba_gqa_gmlp_moe_adaln_diffusion_step'


def rms_normalize(x):
    xf = x.astype(jnp.float32)
    return (xf * lax.rsqrt(jnp.mean(xf * xf, axis=-1, keepdims=True) + EPS)).astype(x.dtype)


def modulation(cvec, w_mod, b_mod):
    m = jax.nn.silu(cvec) @ w_mod + b_mod
    return [p[:, None, :] for p in jnp.split(m, N_MOD, axis=-1)]


def modulate(x, gain, shift, scale):
    return rms_normalize(x) * gain * (1.0 + scale) + shift


def axial_rope_tables(n_tokens):
    n_rows = n_tokens // GRID_W
    row = jnp.repeat(jnp.arange(n_rows, dtype=jnp.float32), GRID_W)
    col = jnp.tile(jnp.arange(GRID_W, dtype=jnp.float32), n_rows)
    half = AXIS_DIM // 2
    inv_freq = ROPE_THETA ** (-jnp.arange(half, dtype=jnp.float32) / half)
    ang_r = row[:, None] * inv_freq[None, :]
    ang_c = col[:, None] * inv_freq[None, :]
    return (jnp.cos(ang_r), jnp.sin(ang_r), jnp.cos(ang_c), jnp.sin(ang_c))


def rotate_axis(x, cos, sin):
    x1, x2 = x[..., :AXIS_DIM // 2], x[..., AXIS_DIM // 2:]
    cos = cos[None, :, None, :]
    sin = sin[None, :, None, :]
    return jnp.concatenate([x1 * cos - x2 * sin, x2 * cos + x1 * sin], axis=-1)


def apply_axial_rope(x, tables):
    cos_r, sin_r, cos_c, sin_c = tables
    xf = x.astype(jnp.float32)
    out = jnp.concatenate([rotate_axis(xf[..., :AXIS_DIM], cos_r, sin_r),
                           rotate_axis(xf[..., AXIS_DIM:], cos_c, sin_c)], axis=-1)
    return out.astype(x.dtype)


def attend(q, k, v):
    B, S = q.shape[0], q.shape[1]
    nb = S // Q_BLOCK
    qb = q.reshape(B, nb, Q_BLOCK, KV_HEADS, GQA_GROUP, HEAD_DIM).transpose(1, 0, 2, 3, 4, 5)
    scale = HEAD_DIM ** -0.5

    def one_block(q_blk):
        s = jnp.einsum('bqkgd,blkd->bkgql', q_blk, k).astype(jnp.float32) * scale
        p = jax.nn.softmax(s, axis=-1).astype(v.dtype)
        return jnp.einsum('bkgql,blkd->bqkgd', p, v)

    o = lax.map(one_block, qb)
    return o.transpose(1, 0, 2, 3, 4, 5).reshape(B, S, ATTN_W)


def chunk_mlp(u, g, w_sp, b_sp):
    B, S = u.shape[0], u.shape[1]
    nc = S // CHUNK
    gc = rms_normalize(g).reshape(B, nc, CHUNK, G_HEADS, G_DIM)
    sp = jnp.einsum('hpq,bnqhd->bnphd', w_sp, gc) + b_sp.T[None, None, :, :, None]
    return (u * sp.reshape(B, S, G_HEADS, G_DIM)).reshape(B, S, GMLP_W)


def mixer(h, k_ctx, v_ctx, tables, w_in, q_gain, k_gain, w_sp, b_sp, attn_out_gain, gmlp_out_gain, w_out):
    B, S, _ = h.shape
    z = jnp.einsum('bsd,de->bse', h, w_in)
    q, k, v, u, g = jnp.split(z, [ATTN_W, ATTN_W + KV_W, ATTN_W + 2 * KV_W, ATTN_W + 2 * KV_W + GMLP_W], axis=-1)
    q = rms_normalize(q.reshape(B, S, N_HEADS, HEAD_DIM)) * q_gain
    k = rms_normalize(k.reshape(B, S, KV_HEADS, HEAD_DIM)) * k_gain
    v = v.reshape(B, S, KV_HEADS, HEAD_DIM)
    u = jax.nn.gelu(u).reshape(B, S, G_HEADS, G_DIM)
    g = jax.nn.gelu(g).reshape(B, S, G_HEADS, G_DIM)
    if tables is not None:
        q = apply_axial_rope(q, tables)
        k = apply_axial_rope(k, tables)
    if k_ctx is not None:
        k_all = jnp.concatenate([k_ctx, k], axis=1)
        v_all = jnp.concatenate([v_ctx, v], axis=1)
    else:
        k_all, v_all = k, v
    attn = attend(q, k_all, v_all)
    gm = chunk_mlp(u, g, w_sp, b_sp)
    merged = jnp.concatenate([rms_normalize(attn) * attn_out_gain, rms_normalize(gm) * gmlp_out_gain], axis=-1)
    return jnp.einsum('bse,ed->bsd', merged, w_out), k, v


def moe_ffn(h, w_router, b_router, w_gate, b_gate, w_up, b_up, w_down, b_down):
    T, D = h.shape
    logits = (h @ w_router).astype(jnp.float32) + b_router.astype(jnp.float32)
    top_val, top_idx = lax.top_k(logits, TOP_K)
    gates = jax.nn.softmax(top_val, axis=-1).astype(h.dtype)
    n_assign = T * TOP_K
    flat_e = top_idx.reshape(-1).astype(jnp.int32)
    flat_tok = jnp.arange(n_assign, dtype=jnp.int32) // TOP_K
    flat_gate = gates.reshape(-1)
    order = jnp.argsort(flat_e)
    sorted_e = flat_e[order]
    counts = jnp.bincount(flat_e, length=N_EXPERTS).astype(jnp.int32)
    padded = ((counts + MOE_BLOCK - 1) // MOE_BLOCK) * MOE_BLOCK
    start = jnp.cumsum(counts) - counts
    pad_end = jnp.cumsum(padded)
    pad_start = pad_end - padded
    dest = pad_start[sorted_e] + jnp.arange(n_assign, dtype=jnp.int32) - start[sorted_e]
    n_blocks = (n_assign + N_EXPERTS * (MOE_BLOCK - 1) + MOE_BLOCK - 1) // MOE_BLOCK
    n_pad = n_blocks * MOE_BLOCK
    tok_buf = jnp.full((n_pad,), T, jnp.int32).at[dest].set(flat_tok[order])
    gate_buf = jnp.zeros((n_pad,), h.dtype).at[dest].set(flat_gate[order])
    block_start = jnp.arange(n_blocks, dtype=jnp.int32) * MOE_BLOCK
    block_e = jnp.minimum(jnp.sum((pad_end[None, :] <= block_start[:, None]).astype(jnp.int32), axis=1), N_EXPERTS - 1)
    h_pad = jnp.concatenate([h, jnp.zeros((1, D), h.dtype)], axis=0)
    xb = h_pad[tok_buf].reshape(n_blocks, MOE_BLOCK, D)

    def expert_block(args):
        x_blk, e = args
        gt = x_blk @ w_gate[e] + b_gate[e]
        up = x_blk @ w_up[e] + b_up[e]
        gt = jnp.minimum(gt, SWIGLU_LIMIT)
        up = jnp.clip(up, -SWIGLU_LIMIT, SWIGLU_LIMIT)
        act = (up + 1.0) * (gt * jax.nn.sigmoid(SWIGLU_ALPHA * gt))
        return act @ w_down[e] + b_down[e]

    yb = lax.map(expert_block, (xb, block_e))
    y = yb.reshape(n_pad, D) * gate_buf[:, None]
    return jax.ops.segment_sum(y, tok_buf, num_segments=T + 1)[:T]


def setup_inputs(seed: int = 0) -> dict:
    key = jax.random.key(seed)
    ks = jax.random.split(key, 28)

    def nrm(k, shape, s):
        return jax.random.normal(k, shape, jnp.float32) * s

    def gain(k, shape):
        return 1.0 + nrm(k, shape, 0.02)

    return {
        'x_prompt': nrm(ks[0], (BATCH, SEQ, D_MODEL), 1.0),
        'x_sample': nrm(ks[1], (DEC_BATCH, DEC_SEQ, D_MODEL), 1.0),
        'cache_k': nrm(ks[2], (DEC_BATCH, DEPTH, PAST_LEN, KV_HEADS, HEAD_DIM), 1.0),
        'cache_v': nrm(ks[3], (DEC_BATCH, DEPTH, PAST_LEN, KV_HEADS, HEAD_DIM), 1.0),
        'c': nrm(ks[4], (DEC_BATCH, D_MODEL), 1.0),
        'c_ctx': nrm(ks[5], (D_MODEL,), 1.0),
        'w_mod': nrm(ks[6], (DEPTH, D_MODEL, N_MOD * D_MODEL), 0.5 * D_MODEL ** -0.5),
        'b_mod': nrm(ks[7], (DEPTH, N_MOD * D_MODEL), 0.01),
        'norm1': gain(ks[8], (DEPTH, D_MODEL)),
        'w_in': nrm(ks[9], (DEPTH, D_MODEL, IN_W), D_MODEL ** -0.5),
        'q_gain': gain(ks[10], (DEPTH, HEAD_DIM)),
        'k_gain': gain(ks[11], (DEPTH, HEAD_DIM)),
        'w_sp': nrm(ks[12], (DEPTH, G_HEADS, CHUNK, CHUNK), CHUNK ** -0.5),
        'b_sp': gain(ks[13], (DEPTH, G_HEADS, CHUNK)),
        'attn_out_gain': gain(ks[14], (DEPTH, ATTN_W)),
        'gmlp_out_gain': gain(ks[15], (DEPTH, GMLP_W)),
        'w_out': nrm(ks[16], (DEPTH, D_MIX, D_MODEL), D_MIX ** -0.5),
        'norm2': gain(ks[17], (DEPTH, D_MODEL)),
        'w_router': nrm(ks[18], (DEPTH, D_MODEL, N_EXPERTS), D_MODEL ** -0.5),
        'b_router': nrm(ks[19], (DEPTH, N_EXPERTS), 0.01),
        'w_gate': nrm(ks[20], (DEPTH, N_EXPERTS, D_MODEL, D_FF), D_MODEL ** -0.5),
        'b_gate': nrm(ks[21], (DEPTH, N_EXPERTS, D_FF), 0.01),
        'w_up': nrm(ks[22], (DEPTH, N_EXPERTS, D_MODEL, D_FF), D_MODEL ** -0.5),
        'b_up': nrm(ks[23], (DEPTH, N_EXPERTS, D_FF), 0.01),
        'w_down': nrm(ks[24], (DEPTH, N_EXPERTS, D_FF, D_MODEL), D_FF ** -0.5),
        'b_down': nrm(ks[25], (DEPTH, N_EXPERTS, D_MODEL), 0.01),
        'norm_f': gain(ks[26], (D_MODEL,)),
    }


def reference(x_prompt, x_sample, cache_k, cache_v, c, c_ctx, w_mod, b_mod, norm1, w_in, q_gain, k_gain,
              w_sp, b_sp, attn_out_gain, gmlp_out_gain, w_out, norm2, w_router, b_router,
              w_gate, b_gate, w_up, b_up, w_down, b_down, norm_f):
    xp, xs = x_prompt, x_sample
    Bp, Sp = xp.shape[0], xp.shape[1]
    tables = axial_rope_tables(xs.shape[1])
    new_k, new_v = [], []
    for l in range(DEPTH):
        sh1p, sc1p, g1p, sh2p, sc2p, g2p = modulation(c_ctx[None, :], w_mod[l], b_mod[l])
        sh1s, sc1s, g1s, sh2s, sc2s, g2s = modulation(c, w_mod[l], b_mod[l])
        mix_w = (w_in[l], q_gain[l], k_gain[l], w_sp[l], b_sp[l], attn_out_gain[l], gmlp_out_gain[l], w_out[l])
        hp = modulate(xp, norm1[l], sh1p, sc1p)
        op, kp, vp = mixer(hp, None, None, None, *mix_w)
        hs = modulate(xs, norm1[l], sh1s, sc1s)
        os_, _, _ = mixer(hs, cache_k[:, l], cache_v[:, l], tables, *mix_w)
        xp = xp + g1p * op
        xs = xs + g1s * os_
        new_k.append(kp)
        new_v.append(vp)
        hp2 = modulate(xp, norm2[l], sh2p, sc2p).reshape(-1, D_MODEL)
        hs2 = modulate(xs, norm2[l], sh2s, sc2s).reshape(-1, D_MODEL)
        f = moe_ffn(jnp.concatenate([hp2, hs2], axis=0), w_router[l], b_router[l], w_gate[l], b_gate[l],
                    w_up[l], b_up[l], w_down[l], b_down[l])
        n_p = Bp * Sp
        xp = xp + g2p * f[:n_p].reshape(xp.shape)
        xs = xs + g2s * f[n_p:].reshape(xs.shape)
    y_prompt = rms_normalize(xp) * norm_f
    y_sample = rms_normalize(xs) * norm_f
    new_cache_k = jnp.stack(new_k, axis=1)
    new_cache_v = jnp.stack(new_v, axis=1)
    return (y_prompt, y_sample, new_cache_k, new_cache_v)
```

```python
import contextlib
import os
import numpy as np
import concourse.bass as bass
import concourse.mybir as mybir
from concourse.bass_utils import run_bass_kernel_spmd

F32 = mybir.dt.float32
BF16 = mybir.dt.bfloat16
I32 = mybir.dt.int32
U32 = mybir.dt.uint32
ALU = mybir.AluOpType
AF = mybir.ActivationFunctionType
AX = mybir.AxisListType

D = 2048
NE = 32
CAP = 512
NTOK = 1536
NT = 12
EPS = 1e-6
SAME_ENG_SYNC = True
UPTO = 99
DEBUG = False
NOSC = False


def _region(ap):
    name = ap.tensor.name
    pat = ap.ap
    off = int(ap.offset)
    es = mybir.dt.size(ap.dtype)
    sp = str(ap.space)
    if sp in ("SB", "PSUM"):
        pstride, npart = pat[0]
        if pstride == 0:
            pstride = 1 << 40
        p0 = off // pstride
        f0 = off % pstride
        ext = 1 + sum((cnt - 1) * abs(step) for step, cnt in pat[1:])
        if sp == "PSUM":
            b0 = (f0 * es) // 2048 * 2048
            b1 = ((f0 + ext) * es + 2047) // 2048 * 2048
            return (name, 0, 128, b0, b1)
        return (name, p0, p0 + npart, f0 * es, (f0 + ext) * es)
    ext = 1 + sum((cnt - 1) * abs(step) for step, cnt in pat)
    return (name, 0, 1, off * es, (off + ext) * es)


class _Op:
    __slots__ = ("eng", "fn", "is_dma", "dkey", "waits", "signal", "sigidx", "dcount", "pos", "deps", "dslot")


class Sched:
    ENGS = ("pe", "act", "dve", "pool", "sp")
    NDSEM = 20

    def __init__(self, nc):
        self.nc = nc
        self.ops = []
        self.acc = {}
        self.dcount = {}
        self.notrack = set()

    def add(self, eng, fn, reads=(), writes=(), dkey=None):
        op = _Op()
        op.eng = eng
        op.fn = fn
        op.is_dma = dkey is not None
        op.dkey = dkey
        op.signal = False
        op.sigidx = 0
        op.waits = []
        op.deps = set()
        idx = len(self.ops)
        if op.is_dma:
            n = self.dcount.get(eng, 0)
            self.dcount[eng] = n + 1
            op.dslot = (eng, n % self.NDSEM)
            op.dcount = n // self.NDSEM + 1
        for is_w, aps in ((False, reads), (True, writes)):
            for ap in aps:
                r = ap if isinstance(ap, tuple) else _region(ap)
                name = r[0]
                if name in self.notrack:
                    continue
                lst = self.acc.get(name, [])
                keep = []
                for a in lst:
                    ov = a[2] < r[2] and r[1] < a[3] and a[4] < r[4] and r[3] < a[5]
                    if ov and (a[1] or is_w) and a[0] != idx:
                        op.deps.add(a[0])
                    if is_w and ov and a[0] != idx and r[1] <= a[2] and a[3] <= r[2] and r[3] <= a[4] and a[5] <= r[4]:
                        continue
                    keep.append(a)
                keep.append([idx, is_w, r[1], r[2], r[3], r[4]])
                self.acc[name] = keep
        self.ops.append(op)
        return idx

    def finish(self, final_eng="sp"):
        nc = self.nc
        ops = self.ops
        cnt = {e: 0 for e in self.ENGS}
        for op in ops:
            cnt[op.eng] += 1
            op.pos = cnt[op.eng]
        waited = {e: {} for e in self.ENGS}
        edges = []
        prev_on_slot = {}
        for i, op in enumerate(ops):
            E = op.eng
            if op.is_dma:
                if op.dslot in prev_on_slot:
                    op.deps.add(prev_on_slot[op.dslot])
                prev_on_slot[op.dslot] = i
            for j in sorted(op.deps):
                p = ops[j]
                if p.is_dma:
                    key = ("d", p.dslot)
                    val = p.dcount
                else:
                    if p.eng == E and (E == "pe" or not SAME_ENG_SYNC):
                        continue
                    key = ("e", p.eng)
                    val = p.pos
                if waited[E].get(key, 0) >= val:
                    continue
                waited[E][key] = val
                if not p.is_dma:
                    p.signal = True
                edges.append((i, key, j))
        sc = {e: 0 for e in self.ENGS}
        for op in ops:
            if op.signal and not op.is_dma:
                sc[op.eng] += 1
                op.sigidx = sc[op.eng]
        stack = contextlib.ExitStack()
        esem = {e: stack.enter_context(nc.semaphore("es_" + e)) for e in self.ENGS}
        slots = {}
        for op in ops:
            if op.is_dma:
                slots[op.dslot] = max(slots.get(op.dslot, 0), op.dcount)
        dsem = {k: stack.enter_context(nc.semaphore("ds_%s_%d" % k)) for k in slots}
        for (i, key, j) in edges:
            p = ops[j]
            if key[0] == "d":
                ops[i].waits.append((dsem[p.dslot], 16 * p.dcount))
            else:
                ops[i].waits.append((esem[p.eng], p.sigidx))
        for op in ops:
            if op.is_dma and op.dcount > 1 and waited[op.eng].get(("d", op.dslot), 0) < op.dcount - 1:
                pass
        final_waits = [(dsem[k], 16 * c) for k, c in slots.items()]
        by_eng = {e: [op for op in ops if op.eng == e] for e in self.ENGS}
        self.stats = {e: len(by_eng[e]) for e in self.ENGS}
        self.stats["edges"] = len(edges)
        self.stats["dsems"] = len(dsem)

        def replay(ename, e):
            for op in by_eng[ename]:
                for (s, v) in op.waits:
                    e.wait_ge(s, v)
                ins = op.fn(e)
                if op.is_dma:
                    ins.then_inc(dsem[op.dslot], 16)
                elif op.signal:
                    ins.then_inc(esem[ename], 1)
            if ename == final_eng:
                for (s, v) in final_waits:
                    e.wait_ge(s, v)

        with nc.Block() as block:
            @block.tensor
            def _(e):
                replay("pe", e)

            @block.scalar
            def _(e):
                replay("act", e)

            @block.vector
            def _(e):
                replay("dve", e)

            @block.gpsimd
            def _(e):
                replay("pool", e)

            @block.sync
            def _(e):
                replay("sp", e)
        stack.close()


class Arena:
    def __init__(self, base, nbytes):
        self.base = base
        self.free = [(0, nbytes)]
        self.live = {}
        self.peak = 0

    def alloc(self, shape, dtype=F32):
        es = mybir.dt.size(dtype)
        n = 1
        for s in shape[1:]:
            n *= s
        nb = (n * es + 63) // 64 * 64
        for i, (o, l) in enumerate(self.free):
            if l >= nb:
                if l == nb:
                    self.free.pop(i)
                else:
                    self.free[i] = (o + nb, l - nb)
                break
        else:
            raise RuntimeError("arena full: want %d, free %s" % (nb, self.free))
        self.peak = max(self.peak, o + nb)
        v = self.base[0:shape[0], o // 4:(o + nb) // 4]
        if dtype != F32:
            v = v.bitcast(dtype)
        v = v[:, 0:n]
        if len(shape) == 3:
            v = v.rearrange("p (a b) -> p a b", a=shape[1])
        elif len(shape) == 4:
            v = v.rearrange("p (a b c) -> p a b c", a=shape[1], b=shape[2])
        self.live[id(v)] = (o, nb, v)
        return v

    def release(self, v):
        o, nb, _ = self.live.pop(id(v))
        self.free.append((o, nb))
        self.free.sort()
        merged = []
        for (a, l) in self.free:
            if merged and merged[-1][0] + merged[-1][1] == a:
                merged[-1] = (merged[-1][0], merged[-1][1] + l)
            else:
                merged.append((a, l))
        self.free = merged


def build_nc(debug=False, upto=99):
    nc = bass.Bass("TRN2", target_bir_lowering=False)

    def din(name, shape, dt=F32):
        return nc.dram_tensor(name, shape, dt, kind="ExternalInput").ap()

    def dout(name, shape, dt=F32):
        return nc.dram_tensor(name, shape, dt, kind="ExternalOutput").ap()

    x_own = din("x_own", [NTOK, D])
    x_kv = din("x_kv", [4096, D])
    ck = din("ck", [256, 256])
    cv = din("cv", [256, 256])
    cT = din("cT", [128, 16, 2])
    w_mod = din("w_mod", [D, 6 * D])
    b_mod = din("b_mod", [1, 6 * D])
    norm1 = din("norm1", [1, D])
    norm2 = din("norm2", [1, D])
    norm_f = din("norm_f", [1, D])
    w_in = din("w_in", [D, 3584])
    w_out = din("w_out", [D, D])
    w_router = din("w_router", [D, NE])
    b_router = din("b_router", [1, NE])
    q_gain = din("q_gain", [1, 128])
    k_gain = din("k_gain", [1, 128])
    w_spT = din("w_spT", [128, 8, 128])
    b_spT = din("b_spT", [128, 8])
    ag = din("ag", [1, 1024])
    gg = din("gg", [1, 1024])
    w_gate = din("w_gate", [NE, D, D])
    w_up = din("w_up", [NE, D, D])
    w_down = din("w_down", [NE, D, D])
    bgT = din("bgT", [128, NE, 16])
    buT = din("buT", [128, NE, 16])
    b_down = din("b_down", [NE, D])
    cos_q = din("cos_q", [1024, 128])
    sin_q = din("sin_q", [1024, 128])
    cos_k = din("cos_k", [4096, 128])
    sin_k = din("sin_k", [4096, 128])

    y_own = dout("y_own", [NTOK, D])
    nk = dout("nk", [512, 256])
    nv = dout("nv", [512, 256])
    dbg = {}
    if debug:
        dbg["x1"] = dout("dbg_x1", [NTOK, D])
        dbg["h2"] = dout("dbg_h2", [NTOK, D])
        dbg["lg"] = dout("dbg_lg", [NTOK, NE])
        dbg["idx"] = dout("dbg_idx", [NTOK, 8], I32)
        dbg["gate"] = dout("dbg_gate", [NTOK, 4])
        dbg["mods"] = dout("dbg_mods", [2, 6, D])
        dbg["f"] = dout("dbg_f", [NTOK, D])

    MODS = nc.dram_tensor("MODS", [2, 6, D], F32, kind="Internal").ap()
    X1 = nc.dram_tensor("X1", [NTOK, D], F32, kind="Internal").ap()
    XS = nc.dram_tensor("XS", [NE * CAP + 128, D], BF16, kind="Internal").ap()
    YB = nc.dram_tensor("YB", [NE * CAP + 128, D], F32, kind="Internal").ap()
    ZROW = NE * CAP

    es = contextlib.ExitStack()
    SBN = 200 * 1024
    SBT = es.enter_context(nc.sbuf_tensor("SB", [128, SBN // 4], F32))
    PS = es.enter_context(nc.psum_tensor("PS", [128, 8, 512], F32))
    A = Arena(SBT[:, :], SBN)
    S = Sched(nc)
    S.notrack.update(["x_own", "x_kv", "ck", "cv", "cT", "w_mod", "b_mod", "norm1", "norm2", "norm_f", "w_in", "w_out",
                      "w_router", "b_router", "q_gain", "k_gain", "w_spT", "b_spT", "ag", "gg", "w_gate", "w_up", "w_down",
                      "bgT", "buT", "b_down", "cos_q", "sin_q", "cos_k", "sin_k"])

    dump_i = [0]
    dump_flag = [0]

    def dump(name, ap, dt=F32):
        if not debug:
            return
        o = nc.dram_tensor("dmp_" + name, list(ap.shape), dt, kind="ExternalOutput").ap()
        dump_i[0] += 1
        S.add("sp", lambda e: e.dma_start(out=o, in_=ap), reads=[ap], writes=[o], dkey="dump%d" % dump_i[0])

    def aps(*xs):
        return [x for x in xs if x is not None and not isinstance(x, (int, float))]

    def dma(eng, out, in_, key):
        S.add(eng, lambda e: e.dma_start(out=out, in_=in_), reads=[in_], writes=[out], dkey=key)

    def mm(out, lhsT, rhs, start=True, stop=True):
        S.add("pe", lambda e: e.matmul(out, lhsT=lhsT, rhs=rhs, start=start, stop=stop), reads=[lhsT, rhs], writes=[out])

    def act(out, in_, func, scale=1.0, bias=0.0, accum=None):
        kw = {}
        if accum is not None:
            kw["accum_out"] = accum
        S.add("act", lambda e: e.activation(out=out, in_=in_, func=func, scale=scale, bias=bias, **kw),
              reads=aps(in_, scale, bias), writes=aps(out, accum))

    def ts(eng, out, in0, s1, s2, op0, op1=None, accum=None):
        kw = {}
        if op1 is not None:
            kw["op1"] = op1
        if accum is not None:
            kw["accum_out"] = accum
        S.add(eng, lambda e: e.tensor_scalar(out=out, in0=in0, scalar1=s1, scalar2=s2, op0=op0, **kw),
              reads=aps(in0, s1, s2), writes=aps(out, accum))

    def tt(eng, out, in0, in1, op):
        S.add(eng, lambda e: e.tensor_tensor(out=out, in0=in0, in1=in1, op=op), reads=[in0, in1], writes=[out])

    def stt(eng, out, in0, scalar, in1, op0, op1):
        S.add(eng, lambda e: e.scalar_tensor_tensor(out=out, in0=in0, scalar=scalar, in1=in1, op0=op0, op1=op1),
              reads=aps(in0, scalar, in1), writes=[out])

    def cp(eng, out, in_):
        if eng == "act":
            S.add("act", lambda e: e.copy(out=out, in_=in_), reads=[in_], writes=[out])
        else:
            S.add(eng, lambda e: e.tensor_copy(out=out, in_=in_), reads=[in_], writes=[out])

    def recip(out, in_):
        S.add("dve", lambda e: e.reciprocal(out=out, in_=in_), reads=[in_], writes=[out])

    def memset(eng, out, val):
        S.add(eng, lambda e: e.memset(out, val), writes=[out])

    iof = A.alloc([128, 128])
    iop = A.alloc([128, 1])
    ident = A.alloc([128, 128])
    tri_b = A.alloc([128, 128], BF16)
    ones_b = A.alloc([128, 128], BF16)
    ones_f = A.alloc([128, 128])
    epsc = A.alloc([128, 1])
    S.add("pool", lambda e: e.iota(iof, pattern=[[1, 128]], base=0, channel_multiplier=0, allow_small_or_imprecise_dtypes=True), writes=[iof])
    S.add("pool", lambda e: e.iota(iop, pattern=[[0, 1]], base=0, channel_multiplier=1, allow_small_or_imprecise_dtypes=True), writes=[iop])
    ts("dve", ident, iof, iop[:, 0:1], None, ALU.is_equal)
    ts("dve", tri_b, iof, iop[:, 0:1], None, ALU.is_gt)
    memset("dve", ones_b, 1.0)
    memset("dve", ones_f, 1.0)
    memset("dve", epsc, EPS)
    c7 = A.alloc([128, 1])
    cm6 = A.alloc([128, 1])
    memset("dve", c7, 7.0)
    memset("dve", cm6, -6.0)
    tz = A.alloc([128, 1])
    ts("dve", tz, iop, float(NE * CAP), None, ALU.add)

    def tr(out, in_):
        k = in_.shape[0]
        S.add("pe", lambda e: e.transpose(out=out, in_=in_, identity=ident[0:k, 0:k]), reads=[in_, ident[0:k, 0:k]], writes=[out])

    evac_rr = [0]

    def evac(out, in_):
        evac_rr[0] += 1
        cp("act" if evac_rr[0] % 2 else "dve", out, in_)

    def rstd_from_ss(rs, ss, n):
        act(rs, ss, AF.Sqrt, scale=1.0 / n, bias=epsc[:, 0:1])
        recip(rs, rs)

    def bc_load(dst, src_row, key="bc"):
        dma("sp", dst, src_row.to_broadcast([128, src_row.shape[-1]]), key)

    def emit():
        wring = [A.alloc([128, 16, 512], BF16) for _ in range(3)]
        wr_i = [0]

        def wload(src2d):
            slot = wr_i[0] % len(wring)
            wr_i[0] += 1
            w = wring[slot]
            dma("pool", w, src2d.rearrange("(kc p) n -> p kc n", p=128), "w%d" % slot)
            return w

        zb = A.alloc([128, 8192], BF16)
        memset("dve", zb, 0.0)
        for i in range(NE):
            dma("sp", XS[i * CAP:(i + 1) * CAP, :].rearrange("(p t) d -> p (t d)", p=128), zb, "xsz")
        dma("sp", XS[NE * CAP:NE * CAP + 128, :], zb[:, 0:D], "xsz")
        cT_sb = A.alloc([128, 16, 2])
        sc_b = A.alloc([128, 16, 2], BF16)
        dma("sp", cT_sb, cT, "misc")
        act(sc_b, cT_sb, AF.Silu)
        nrm = A.alloc([2, 2, D])
        for j in range(2):
            dma("sp", nrm[j:j + 1, 0, :], norm1, "misc")
            dma("sp", nrm[j:j + 1, 1, :], norm2, "misc")
        bm = [A.alloc([1, 512]) for _ in range(2)]
        mrow = [A.alloc([2, 512]) for _ in range(2)]
        for pc in range(24):
            comp, cb = pc // 4, (pc % 4) * 512
            w = wload(w_mod[:, pc * 512:(pc + 1) * 512])
            bmt = bm[pc % 2]
            dma("sp", bmt, b_mod[0:1, pc * 512:(pc + 1) * 512], "bm%d" % (pc % 2))
            pb = PS[0:2, pc % 2, :]
            for kc in range(16):
                mm(pb, sc_b[:, kc, :], w[:, kc, :], start=(kc == 0), stop=False)
            mm(pb, ones_f[0:1, 0:2], bmt, start=False, stop=True)
            mr = mrow[pc % 2]
            if comp in (1, 4):
                stt("dve", mr, pb, 1.0, nrm[:, 0 if comp == 1 else 1, cb:cb + 512], ALU.add, ALU.mult)
            else:
                cp("dve", mr, pb)
            dma("sp", MODS[:, comp, cb:cb + 512], mr, "mods")
        if debug:
            dtmp = A.alloc([2, 6, D])
            dma("sp", dtmp, MODS, "dbgl")
            dma("sp", dbg["mods"], dtmp, "dbg")
            A.release(dtmp)
        for t_ in (zb, cT_sb, sc_b, nrm, bm[0], bm[1], mrow[0], mrow[1]):
            A.release(t_)

        psr = [0]

        def bank(i=None):
            if i is None:
                psr[0] += 1
                i = psr[0] % 8
            return PS[:, i, :]

        def make_h(src_rows, Abc, Bbc, hT_dst, col0):
            xt = A.alloc([128, D])
            hb = A.alloc([128, D])
            ss = A.alloc([128, 2])
            dma("sp", xt, src_rows, "xt")
            act(hb, xt, AF.Square, accum=ss[:, 0:1])
            rstd_from_ss(ss[:, 1:2], ss[:, 0:1], D)
            act(hb, xt, AF.Identity, scale=ss[:, 1:2])
            tt("dve", hb, hb, Abc, ALU.mult)
            tt("pool", hb, hb, Bbc, ALU.add)
            if dump_flag[0] == 1:
                dump("hb", hb)
                dump("xt", xt)
                dump("ss", ss)
            for g4 in range(4):
                pb = bank()
                for j in range(4):
                    kc = g4 * 4 + j
                    tr(pb[:, j * 128:(j + 1) * 128], hb[:, kc * 128:(kc + 1) * 128])
                evac(hT_dst[:, g4 * 4:(g4 + 1) * 4, col0:col0 + 128], pb.rearrange("p (a b) -> p a b", a=4))
            A.release(xt)
            A.release(hb)
            A.release(ss)

        def heads_post(pb, nh, gain_bc, cosT, sinT, dstT_list, f32_out=None):
            ss = A.alloc([128, 2 * nh])
            junk = A.alloc([128, 128])
            for h in range(nh):
                act(junk, pb[:, h * 128:(h + 1) * 128], AF.Square, accum=ss[:, h:h + 1])
            rstd_from_ss(ss[:, nh:2 * nh], ss[:, 0:nh], 128)
            for h in range(nh):
                kn = A.alloc([128, 128]) if f32_out is None else f32_out[h]
                stt("dve", kn, pb[:, h * 128:(h + 1) * 128], ss[:, nh + h:nh + h + 1], gain_bc, ALU.mult, ALU.mult)
                if cosT is not None:
                    t1 = A.alloc([128, 128])
                    t2 = A.alloc([128, 128])
                    tt("dve", t1, kn, cosT, ALU.mult)
                    kn4 = kn.rearrange("p (a b c) -> p a b c", a=2, b=2)
                    t24 = t2.rearrange("p (a b c) -> p a b c", a=2, b=2)
                    sn4 = sinT.rearrange("p (a b c) -> p a b c", a=2, b=2)
                    tt("pool", t24[:, :, 0, :], kn4[:, :, 1, :], sn4[:, :, 0, :], ALU.mult)
                    tt("pool", t24[:, :, 1, :], kn4[:, :, 0, :], sn4[:, :, 1, :], ALU.mult)
                    tt("dve", t1, t1, t2, ALU.add)
                    src = t1
                else:
                    src = kn
                pt = bank()
                tr(pt[:, 0:128], src)
                evac(dstT_list[h], pt[:, 0:128])
                if cosT is not None:
                    A.release(t1)
                    A.release(t2)
                if f32_out is None:
                    A.release(kn)
            A.release(ss)
            A.release(junk)

        if upto < 1:
            return
        kT_all = A.alloc([128, 2, 4352], BF16)
        V_all = A.alloc([128, 34, 2, 132], BF16)
        memset("pool", V_all, 1.0)
        kg_bc = A.alloc([128, 128])
        qg_bc = A.alloc([128, 128])
        bc_load(kg_bc, k_gain[0:1, :])
        bc_load(qg_bc, q_gain[0:1, :])
        ckt = A.alloc([128, 2, 256])
        dma("sp", ckt, ck.rearrange("(t p) f -> p t f", p=128), "misc")
        cvt = A.alloc([128, 2, 256])
        dma("sp", cvt, cv.rearrange("(t p) f -> p t f", p=128), "misc")
        for t in range(2):
            cp("dve", V_all[:, t, :, 0:128], cvt[:, t, :].rearrange("p (h d) -> p h d", h=2))
        A.release(cvt)
        for t in range(2):
            for h in range(2):
                pt = bank()
                tr(pt[:, 0:128], ckt[:, t, h * 128:(h + 1) * 128])
                evac(kT_all[:, h, t * 128:(t + 1) * 128], pt[:, 0:128])
        A.release(ckt)

        A1s = A.alloc([128, D])
        B1s = A.alloc([128, D])
        bc_load(A1s, MODS[1:2, 1, :])
        bc_load(B1s, MODS[1:2, 0, :])
        w_kv = wload(w_in[:, 1024:1536])
        hTk = [A.alloc([128, 16, 128], BF16) for _ in range(2)]
        for i in range(32):
            hT = hTk[i % 2]
            make_h(x_kv[i * 128:(i + 1) * 128, :], A1s, B1s, hT, 0)
            pb = bank()
            for kc in range(16):
                mm(pb, hT[:, kc, :], w_kv[:, kc, :], start=(kc == 0), stop=(kc == 15))
            cosT = A.alloc([128, 128])
            sinT = A.alloc([128, 128])
            dma("sp", cosT, cos_k[i * 128:(i + 1) * 128, :], "rope")
            dma("sp", sinT, sin_k[i * 128:(i + 1) * 128, :], "rope")
            heads_post(pb, 2, kg_bc, cosT, sinT, [kT_all[:, h, 256 + i * 128:256 + (i + 1) * 128] for h in range(2)])
            evac(V_all[:, 2 + i, :, 0:128], pb[:, 256:512].rearrange("p (h d) -> p h d", h=2))
            A.release(cosT)
            A.release(sinT)
        for t_ in hTk:
            A.release(t_)

        if upto < 2:
            return
        A.release(A1s)
        A.release(B1s)
        ag_bc = A.alloc([128, 1024])
        gg_bc = A.alloc([128, 1024])
        bc_load(ag_bc, ag[0:1, :])
        bc_load(gg_bc, gg[0:1, :])
        wsp_b = A.alloc([128, 8, 128], BF16)
        dma("pool", wsp_b, w_spT, "misc2")
        bsp = A.alloc([128, 8])
        dma("sp", bsp, b_spT, "misc")
        wr_sb = A.alloc([128, 16, NE])
        dma("sp", wr_sb, w_router.rearrange("(kc p) n -> p kc n", p=128), "misc")
        br_sb = A.alloc([1, NE])
        dma("sp", br_sb, b_router, "misc")
        maskb = A.alloc([128, NT, NE], BF16)
        idx_all = A.alloc([128, NT, 8], I32)
        gate_all = A.alloc([128, NT, 4])
        gmT_all = A.alloc([NE, NT, 128])
        scale_att = 128.0 ** -0.5
        if 2 <= upto <= 2.01:
            return

        for blk in range(6):
            is_p = blk < 2
            g = 0 if is_p else 1
            Abc = A.alloc([128, D])
            Bbc = A.alloc([128, D])
            bc_load(Abc, MODS[g:g + 1, 1, :])
            bc_load(Bbc, MODS[g:g + 1, 0, :])
            tiles = [2 * blk, 2 * blk + 1]
            hTb = A.alloc([128, 16, 256], BF16)
            for j, t in enumerate(tiles):
                dump_flag[0] = 1 if t == 0 else 0
                make_h(x_own[t * 128:(t + 1) * 128, :], Abc, Bbc, hTb, j * 128)
            dump_flag[0] = 0
            if blk == 0:
                dump("hTb", hTb, BF16)
            A.release(Abc)
            A.release(Bbc)
            if 2 <= upto <= 2.02:
                return
            qTb = A.alloc([128, 8, 256], BF16)
            if is_p:
                kTp = A.alloc([128, 2, 256], BF16)
                Vp = A.alloc([128, 2, 2, 132], BF16)
                memset("pool", Vp, 1.0)
            for pi in range(3 if is_p else 2):
                if 2 <= upto <= 2.03 + 0.01 * pi - 0.001:
                    return
                w = wload(w_in[:, pi * 512:(pi + 1) * 512])
                for j, t in enumerate(tiles):
                    pb = bank()
                    for kc in range(16):
                        mm(pb, hTb[:, kc, j * 128:(j + 1) * 128], w[:, kc, :], start=(kc == 0), stop=(kc == 15))
                    if pi < 2:
                        if is_p:
                            cosT = sinT = None
                        else:
                            cosT = A.alloc([128, 128])
                            sinT = A.alloc([128, 128])
                            r0 = (t - 4) * 128
                            dma("sp", cosT, cos_q[r0:r0 + 128, :], "rope")
                            dma("sp", sinT, sin_q[r0:r0 + 128, :], "rope")
                        heads_post(pb, 4, qg_bc, cosT, sinT, [qTb[:, pi * 4 + h, j * 128:(j + 1) * 128] for h in range(4)])
                        if cosT is not None:
                            A.release(cosT)
                            A.release(sinT)
                    else:
                        kfs = [A.alloc([128, 128]) for _ in range(2)]
                        heads_post(pb, 2, kg_bc, None, None, [kTp[:, h, j * 128:(j + 1) * 128] for h in range(2)], f32_out=kfs)
                        for h in range(2):
                            dma("sp", nk[t * 128:(t + 1) * 128, h * 128:(h + 1) * 128], kfs[h], "nk")
                        vf = A.alloc([128, 256])
                        cp("act", vf, pb[:, 256:512])
                        dma("sp", nv[t * 128:(t + 1) * 128, :], vf, "nv")
                        evac(Vp[:, j, :, 0:128], pb[:, 256:512].rearrange("p (h d) -> p h d", h=2))
                        A.release(kfs[0])
                        A.release(kfs[1])
                        A.release(vf)
            if 2 <= upto <= 2.1:
                return
            at_tok = A.alloc([128, 2, 1024])
            nkt = 2 if is_p else 34
            pTr = [A.alloc([128, 256], BF16) for _ in range(3)]
            for h in range(8):
                hk = h // 4
                pob = 2 + 2 * (h % 2)

                def kt_ap(kt):
                    return (kTp[:, hk, kt * 128:(kt + 1) * 128] if is_p else kT_all[:, hk, kt * 128:(kt + 1) * 128])

                def v_ap(kt):
                    return (Vp[:, kt, hk, 0:129] if is_p else V_all[:, kt, hk, 0:129])

                def score(kt):
                    mm(PS[:, 6 + (kt % 2), 0:256], kt_ap(kt), qTb[:, h, :])

                score(0)
                if nkt > 1:
                    score(1)
                for kt in range(nkt):
                    pT = pTr[kt % 3]
                    act(pT, PS[:, 6 + (kt % 2), 0:256], AF.Exp, scale=scale_att)
                    for j in range(2):
                        mm(PS[:, pob + j, 0:129], pT[:, j * 128:(j + 1) * 128], v_ap(kt), start=(kt == 0), stop=(kt == nkt - 1))
                    if kt + 2 < nkt:
                        score(kt + 2)
                rs2 = A.alloc([128, 2])
                for j in range(2):
                    recip(rs2[:, j:j + 1], PS[:, pob + j, 128:129])
                    ts("dve", at_tok[:, j, h * 128:(h + 1) * 128], PS[:, pob + j, 0:128], rs2[:, j:j + 1], None, ALU.mult)
                A.release(rs2)
            for t_ in pTr:
                A.release(t_)
            A.release(qTb)
            if is_p:
                A.release(kTp)
                A.release(Vp)
            if 2 <= upto <= 2.2:
                return
            gm_tok = A.alloc([128, 2, 1024])
            for half in range(2):
                wg_ = wload(w_in[:, 2560 + half * 512:2560 + (half + 1) * 512])
                wu_ = wload(w_in[:, 1536 + half * 512:1536 + (half + 1) * 512])
                for j, t in enumerate(tiles):
                    pg = bank()
                    pu = bank()
                    for kc in range(16):
                        mm(pg, hTb[:, kc, j * 128:(j + 1) * 128], wg_[:, kc, :], start=(kc == 0), stop=(kc == 15))
                    for kc in range(16):
                        mm(pu, hTb[:, kc, j * 128:(j + 1) * 128], wu_[:, kc, :], start=(kc == 0), stop=(kc == 15))
                    ggl = A.alloc([128, 512])
                    ugl = A.alloc([128, 512])
                    gcb = A.alloc([128, 4, 128], BF16)
                    ss = A.alloc([128, 8])
                    junk = A.alloc([128, 128])
                    act(ggl, pg, AF.Gelu_apprx_tanh)
                    act(ugl, pu, AF.Gelu_apprx_tanh)
                    for hh in range(4):
                        act(junk, ggl[:, hh * 128:(hh + 1) * 128], AF.Square, accum=ss[:, hh:hh + 1])
                    rstd_from_ss(ss[:, 4:8], ss[:, 0:4], 128)
                    for hh in range(4):
                        ts("dve", gcb[:, hh, :], ggl[:, hh * 128:(hh + 1) * 128], ss[:, 4 + hh:5 + hh], None, ALU.mult)
                    psp = bank()
                    for hh in range(4):
                        mm(psp[:, hh * 128:(hh + 1) * 128], wsp_b[:, half * 4 + hh, :], gcb[:, hh, :])
                    for hh in range(4):
                        hd = half * 4 + hh
                        stt("dve", gm_tok[:, j, hd * 128:(hd + 1) * 128], psp[:, hh * 128:(hh + 1) * 128], bsp[:, hd:hd + 1],
                            ugl[:, hh * 128:(hh + 1) * 128], ALU.add, ALU.mult)
                    for t_ in (ggl, ugl, gcb, ss, junk):
                        A.release(t_)
            A.release(hTb)
            if 2 <= upto <= 2.3:
                return
            mTb = A.alloc([128, 16, 256], BF16)
            for j in range(2):
                for which, src, gbc in ((0, at_tok, ag_bc), (1, gm_tok, gg_bc)):
                    ss = A.alloc([128, 2])
                    mg = A.alloc([128, 1024])
                    act(mg, src[:, j, :], AF.Square, accum=ss[:, 0:1])
                    rstd_from_ss(ss[:, 1:2], ss[:, 0:1], 1024)
                    stt("dve", mg, src[:, j, :], ss[:, 1:2], gbc, ALU.mult, ALU.mult)
                    for g4 in range(2):
                        pb = bank()
                        for jj in range(4):
                            c0 = (g4 * 4 + jj) * 128
                            tr(pb[:, jj * 128:(jj + 1) * 128], mg[:, c0:c0 + 128])
                        evac(mTb[:, which * 8 + g4 * 4:which * 8 + (g4 + 1) * 4, j * 128:(j + 1) * 128], pb.rearrange("p (a b) -> p a b", a=4))
                    A.release(ss)
                    A.release(mg)
            A.release(at_tok)
            A.release(gm_tok)
            if 2 <= upto <= 2.4:
                return
            g1bc = A.alloc([128, D])
            A2bc = A.alloc([128, D])
            B2bc = A.alloc([128, D])
            bc_load(g1bc, MODS[g:g + 1, 2, :])
            bc_load(A2bc, MODS[g:g + 1, 4, :])
            bc_load(B2bc, MODS[g:g + 1, 3, :])
            for n in range(4):
                w = wload(w_out[:, n * 512:(n + 1) * 512])
                for j in range(2):
                    pb = PS[:, j * 4 + n, :]
                    for kc in range(16):
                        mm(pb, mTb[:, kc, j * 128:(j + 1) * 128], w[:, kc, :], start=(kc == 0), stop=(kc == 15))
            for j, t in enumerate(tiles):
                lb_i = [0]

                def lbank(j=j, lb_i=lb_i):
                    lb_i[0] += 1
                    return PS[:, j * 4 + (lb_i[0] % 4), :]

                xt = A.alloc([128, D])
                x1 = A.alloc([128, D])
                dma("sp", xt, x_own[t * 128:(t + 1) * 128, :], "xt2")
                for n in range(4):
                    tt("dve", x1[:, n * 512:(n + 1) * 512], PS[:, j * 4 + n, :], g1bc[:, n * 512:(n + 1) * 512], ALU.mult)
                tt("pool", x1, x1, xt, ALU.add)
                dma("sp", X1[t * 128:(t + 1) * 128, :], x1, "x1w")
                if debug:
                    dma("sp", dbg["x1"][t * 128:(t + 1) * 128, :], x1, "dbg")
                if 2 <= upto <= 2.5:
                    return
                ss = A.alloc([128, 2])
                h2 = xt
                act(h2, x1, AF.Square, accum=ss[:, 0:1])
                rstd_from_ss(ss[:, 1:2], ss[:, 0:1], D)
                act(h2, x1, AF.Identity, scale=ss[:, 1:2])
                tt("dve", h2, h2, A2bc, ALU.mult)
                tt("pool", h2, h2, B2bc, ALU.add)
                if debug:
                    dma("sp", dbg["h2"][t * 128:(t + 1) * 128, :], h2, "dbg")
                h2b = A.alloc([128, D], BF16)
                cp("act", h2b, h2)
                h2T = A.alloc([128, 16, 128])
                for g4 in range(4):
                    pb = lbank()
                    for jj in range(4):
                        kc = g4 * 4 + jj
                        tr(pb[:, jj * 128:(jj + 1) * 128], h2[:, kc * 128:(kc + 1) * 128])
                    evac(h2T[:, g4 * 4:(g4 + 1) * 4, :], pb.rearrange("p (a b) -> p a b", a=4))
                if 2 <= upto <= 2.6:
                    return
                pl = lbank()
                for kc in range(16):
                    mm(pl[:, 0:NE], h2T[:, kc, :], wr_sb[:, kc, :], start=(kc == 0), stop=False)
                mm(pl[:, 0:NE], ones_f[0:1, 0:128], br_sb, start=False, stop=True)
                lg = A.alloc([128, NE])
                cp("dve", lg, pl[:, 0:NE])
                if debug:
                    dma("sp", dbg["lg"][t * 128:(t + 1) * 128, :], lg, "dbg")
                if 2 <= upto <= 2.7:
                    return
                mx = A.alloc([128, 8])
                mi = A.alloc([128, 8], U32)
                ef = A.alloc([128, 8])
                S.add("dve", lambda e, mx=mx, lg=lg: e.max(out=mx, in_=lg), reads=[lg], writes=[mx])
                S.add("dve", lambda e, mx=mx, lg=lg, mi=mi: e.max_index(out=mi, in_max=mx, in_values=lg), reads=[lg, mx], writes=[mi])
                cp("dve", ef, mi)
                ts("dve", maskb[:, t, :], lg, mx[:, 3:4], None, ALU.is_ge)
                nmx = A.alloc([128, 2])
                ts("dve", nmx[:, 0:1], mx[:, 0:1], -1.0, None, ALU.mult)
                ex4 = A.alloc([128, 4])
                act(ex4, mx[:, 0:4], AF.Exp, bias=nmx[:, 0:1], accum=nmx[:, 1:2])
                recip(nmx[:, 1:2], nmx[:, 1:2])
                if 2 <= upto <= 2.8:
                    return
                pp = lbank()
                for i2 in range(t):
                    mm(pp[:, 0:NE], ones_b, maskb[:, i2, :], start=(i2 == 0), stop=False)
                mm(pp[:, 0:NE], tri_b, maskb[:, t, :], start=(t == 0), stop=True)
                posf = A.alloc([128, NE])
                cp("dve", posf, pp[:, 0:NE])
                gm_d = A.alloc([128, NE])
                idxf = A.alloc([128, 8])
                oh = A.alloc([128, NE])
                pr = A.alloc([128, NE])
                pk = A.alloc([128, 4])
                vk = A.alloc([128, 4])
                for k in range(4):
                    ts("dve", oh, iof[:, 0:NE], ef[:, k:k + 1], None, ALU.is_equal)
                    tt("dve", pr, oh, posf, ALU.mult)
                    S.add("dve", lambda e, pk=pk, pr=pr, k=k: e.reduce_sum(out=pk[:, k:k + 1], in_=pr, axis=AX.X), reads=[pr], writes=[pk[:, k:k + 1]])
                    ts("dve", vk[:, k:k + 1], pk[:, k:k + 1], float(CAP), None, ALU.is_lt)
                    stt("dve", gate_all[:, t, k:k + 1], ex4[:, k:k + 1], nmx[:, 1:2], vk[:, k:k + 1], ALU.mult, ALU.mult)
                    if k == 0:
                        ts("dve", gm_d, oh, gate_all[:, t, 0:1], None, ALU.mult)
                    else:
                        stt("dve", gm_d, oh, gate_all[:, t, k:k + 1], gm_d, ALU.mult, ALU.add)
                    stt("dve", idxf[:, k:k + 1], ef[:, k:k + 1], float(CAP), pk[:, k:k + 1], ALU.mult, ALU.add)
                inv = A.alloc([128, 4])
                ts("dve", inv, vk, -1.0, 1.0, ALU.mult, ALU.add)
                tt("dve", idxf[:, 0:4], idxf[:, 0:4], vk, ALU.mult)
                stt("dve", idxf[:, 4:8], inv, tz[:, 0:1], idxf[:, 0:4], ALU.mult, ALU.add)
                stt("dve", idxf[:, 0:4], inv, float(ZROW), idxf[:, 0:4], ALU.mult, ALU.add)
                cp("dve", idx_all[:, t, 0:4], idxf[:, 4:8])
                cp("dve", idx_all[:, t, 4:8], idxf[:, 0:4])
                if debug:
                    dma("sp", dbg["idx"][t * 128:(t + 1) * 128, :], idx_all[:, t, :], "dbg")
                    dma("sp", dbg["gate"][t * 128:(t + 1) * 128, :], gate_all[:, t, :], "dbg")
                pg_ = lbank()
                tr(pg_[0:NE, 0:128], gm_d)
                cp("dve", gmT_all[:, t, :], pg_[0:NE, 0:128])
                for k in range(0 if NOSC else 4):
                    S.add("pool", lambda e, t=t, k=k, h2b=h2b: e.indirect_dma_start(
                        out=XS, out_offset=bass.IndirectOffsetOnAxis(ap=idx_all[:, t, k:k + 1], axis=0),
                        in_=h2b, in_offset=None),
                        reads=[h2b, idx_all[:, t, k:k + 1]], writes=[XS], dkey="xs")
                for t_ in (xt, x1, ss, h2b, h2T, lg, mx, mi, ef, nmx, ex4, posf, gm_d, idxf, oh, pr, pk, vk, inv):
                    A.release(t_)
            for t_ in (mTb, g1bc, A2bc, B2bc):
                A.release(t_)

        for t_ in (kT_all, V_all, kg_bc, qg_bc, ag_bc, gg_bc, wsp_b, bsp, wr_sb, br_sb, maskb):
            A.release(t_)
        for t_ in wring:
            A.release(t_)

        if upto < 3:
            return
        zt = A.alloc([128, D])
        memset("dve", zt, 0.0)
        dma("sp", YB[ZROW:ZROW + 128, :], zt, "ybz")
        A.release(zt)
        bg_sb = A.alloc([128, NE, 16])
        bu_sb = A.alloc([128, NE, 16])
        dma("sp", bg_sb, bgT, "misc")
        dma("sp", bu_sb, buT, "misc")
        ts("dve", bu_sb, bu_sb, 1.0, None, ALU.add)
        ering = [A.alloc([128, 16, 512], BF16) for _ in range(4)]
        er_i = [0]

        def eload(src2d):
            slot = er_i[0] % len(ering)
            er_i[0] += 1
            w = ering[slot]
            dma("pool", w, src2d.rearrange("(kc p) n -> p kc n", p=128), "e%d" % slot)
            return w

        xtk = A.alloc([128, 4, D], BF16)
        xsT2 = [A.alloc([128, 16, CAP], BF16) for _ in range(2)]
        actT2 = [A.alloc([128, 16, CAP], BF16) for _ in range(2)]
        identb = A.alloc([128, 128], BF16)
        cp("dve", identb, ident)

        def prep(e_):
            xsT = xsT2[e_ % 2]
            dma("sp", xtk, XS[e_ * CAP:(e_ + 1) * CAP, :].rearrange("(t p) d -> p t d", p=128), "xtk")
            for kc in range(16):
                pb = bank(kc % 2)
                for tb in range(4):
                    mm(pb[:, tb * 128:(tb + 1) * 128], xtk[:, tb, kc * 128:(kc + 1) * 128], identb)
                evac(xsT[:, kc, :], pb)

        def gate_up(e_):
            xsT = xsT2[e_ % 2]
            actT = actT2[e_ % 2]
            for mg in range(4):
                wg_ = eload(w_gate[e_, :, mg * 512:(mg + 1) * 512])
                wu_ = eload(w_up[e_, :, mg * 512:(mg + 1) * 512])
                for m4 in range(4):
                    mc = mg * 4 + m4
                    pg = bank(2 + (mc % 2))
                    pu = bank(4 + (mc % 2))
                    for kc in range(16):
                        mm(pg, wg_[:, kc, m4 * 128:(m4 + 1) * 128], xsT[:, kc, :], start=(kc == 0), stop=(kc == 15))
                    for kc in range(16):
                        mm(pu, wu_[:, kc, m4 * 128:(m4 + 1) * 128], xsT[:, kc, :], start=(kc == 0), stop=(kc == 15))
                    gt = A.alloc([128, CAP])
                    sg = A.alloc([128, CAP])
                    up = A.alloc([128, CAP])
                    ts("dve", gt, pg, bg_sb[:, e_, mc:mc + 1], c7[:, 0:1], ALU.add, ALU.min)
                    act(sg, gt, AF.Sigmoid, scale=1.702)
                    ts("dve", up, pu, bu_sb[:, e_, mc:mc + 1], cm6[:, 0:1], ALU.add, ALU.max)
                    tt("dve", gt, gt, sg, ALU.mult)
                    stt("dve", actT[:, mc, :], up, 8.0, gt, ALU.min, ALU.mult)
                    for t_ in (gt, sg, up):
                        A.release(t_)

        def down(e_):
            actT = actT2[e_ % 2]
            for n in range(4):
                wd_ = eload(w_down[e_, :, n * 512:(n + 1) * 512])
                for tb in range(4):
                    py = bank(6 + (tb % 2))
                    for fc in range(16):
                        mm(py, actT[:, fc, tb * 128:(tb + 1) * 128], wd_[:, fc, :], start=(fc == 0), stop=(fc == 15))
                    yo = A.alloc([128, 512])
                    cp("act", yo, py)
                    r0 = e_ * CAP + tb * 128
                    dma("sp", YB[r0:r0 + 128, n * 512:(n + 1) * 512], yo, "yb")
                    A.release(yo)

        prep(0)
        for e_ in range(NE):
            gate_up(e_)
            if e_ + 1 < NE:
                prep(e_ + 1)
            down(e_)
        for t_ in (bg_sb, bu_sb, xtk, xsT2[0], xsT2[1], actT2[0], actT2[1], identb):
            A.release(t_)
        for t_ in ering:
            A.release(t_)

        if upto < 4:
            return
        bd_sb = A.alloc([NE, D])
        dma("sp", bd_sb, b_down, "misc")
        nf_bc = A.alloc([128, D])
        bc_load(nf_bc, norm_f[0:1, :])
        g2 = [A.alloc([128, D]) for _ in range(2)]
        bc_load(g2[0], MODS[0:1, 5, :])
        bc_load(g2[1], MODS[1:2, 5, :])
        for t in range(NT):
            g = 0 if t < 4 else 1
            f = A.alloc([128, D])
            for n in range(4):
                mm(PS[:, n, :], gmT_all[:, t, :], bd_sb[:, n * 512:(n + 1) * 512])
            for k in range(4):
                r = A.alloc([128, D])
                S.add("pool", lambda e, t=t, k=k, r=r: e.indirect_dma_start(
                    out=r, out_offset=None, in_=YB, in_offset=bass.IndirectOffsetOnAxis(ap=idx_all[:, t, 4 + k:5 + k], axis=0)),
                    reads=[YB, idx_all[:, t, 4 + k:5 + k]], writes=[r], dkey="yg%d" % (k % 2))
                if k == 0:
                    for n in range(4):
                        stt("dve", f[:, n * 512:(n + 1) * 512], r[:, n * 512:(n + 1) * 512], gate_all[:, t, 0:1], PS[:, n, :], ALU.mult, ALU.add)
                else:
                    stt("dve", f, r, gate_all[:, t, k:k + 1], f, ALU.mult, ALU.add)
                A.release(r)
            if debug:
                dma("sp", dbg["f"][t * 128:(t + 1) * 128, :], f, "dbg")
            x1 = A.alloc([128, D])
            dma("sp", x1, X1[t * 128:(t + 1) * 128, :], "x1r")
            tt("dve", f, f, g2[g], ALU.mult)
            tt("pool", f, f, x1, ALU.add)
            ss = A.alloc([128, 2])
            act(x1, f, AF.Square, accum=ss[:, 0:1])
            rstd_from_ss(ss[:, 1:2], ss[:, 0:1], D)
            stt("dve", x1, f, ss[:, 1:2], nf_bc, ALU.mult, ALU.mult)
            dma("sp", y_own[t * 128:(t + 1) * 128, :], x1, "yout")
            for t_ in (f, x1, ss):
                A.release(t_)


    emit()
    S.finish()
    es.close()
    build_nc.stats = dict(S.stats, sb_peak=A.peak)
    return nc


def _rope_tables():
    n = 4096
    row = np.repeat(np.arange(n // 64, dtype=np.float32), 64)
    col = np.tile(np.arange(64, dtype=np.float32), n // 64)
    inv = (np.float32(10000.0) ** (-np.arange(32, dtype=np.float32) / np.float32(32))).astype(np.float32)
    ar = row[:, None] * inv[None, :]
    ac = col[:, None] * inv[None, :]
    cr, sr, cc, sc_ = np.cos(ar), np.sin(ar), np.cos(ac), np.sin(ac)
    cos4 = np.concatenate([cr, cr, cc, cc], 1).astype(np.float32)
    sin4 = np.concatenate([-sr, sr, -sc_, sc_], 1).astype(np.float32)
    return cos4, sin4


def core_inputs(I, c, shared):
    sb, qt = c // 4, c % 4
    m = dict(shared)
    m["x_own"] = np.ascontiguousarray(np.concatenate(
        [I["x_prompt"][2 * c].reshape(256, D), I["x_prompt"][2 * c + 1].reshape(256, D),
         I["x_sample"][sb, qt * 1024:(qt + 1) * 1024]], 0))
    m["x_kv"] = np.ascontiguousarray(I["x_sample"][sb])
    m["ck"] = np.ascontiguousarray(I["cache_k"][sb, 0].reshape(256, 256))
    m["cv"] = np.ascontiguousarray(I["cache_v"][sb, 0].reshape(256, 256))
    cvecs = np.stack([I["c_ctx"], I["c"][sb]], 1)
    m["cT"] = np.ascontiguousarray(cvecs.reshape(16, 128, 2).transpose(1, 0, 2))
    m["cos_q"] = np.ascontiguousarray(shared["cos_k"][qt * 1024:(qt + 1) * 1024])
    m["sin_q"] = np.ascontiguousarray(shared["sin_k"][qt * 1024:(qt + 1) * 1024])
    return m


def shared_inputs(I):
    cos4, sin4 = _rope_tables()
    s = {
        "w_mod": I["w_mod"][0], "b_mod": I["b_mod"][0].reshape(1, -1), "norm1": I["norm1"][0].reshape(1, -1),
        "norm2": I["norm2"][0].reshape(1, -1), "norm_f": I["norm_f"].reshape(1, -1), "w_in": I["w_in"][0],
        "w_out": I["w_out"][0], "w_router": I["w_router"][0], "b_router": I["b_router"][0].reshape(1, -1),
        "q_gain": I["q_gain"][0].reshape(1, -1), "k_gain": I["k_gain"][0].reshape(1, -1),
        "w_spT": np.ascontiguousarray(I["w_sp"][0].transpose(2, 0, 1)),
        "b_spT": np.ascontiguousarray(I["b_sp"][0].T),
        "ag": I["attn_out_gain"][0].reshape(1, -1), "gg": I["gmlp_out_gain"][0].reshape(1, -1),
        "w_gate": I["w_gate"][0], "w_up": I["w_up"][0], "w_down": I["w_down"][0],
        "bgT": np.ascontiguousarray(I["b_gate"][0].reshape(NE, 16, 128).transpose(2, 0, 1)),
        "buT": np.ascontiguousarray(I["b_up"][0].reshape(NE, 16, 128).transpose(2, 0, 1)),
        "b_down": I["b_down"][0], "cos_k": cos4, "sin_k": sin4,
    }
    return {k: np.ascontiguousarray(np.asarray(v, dtype=np.float32)) for k, v in s.items()}


def kernel(**inputs):
    I = {k: np.asarray(v) for k, v in inputs.items()}
    shared = shared_inputs(I)
    in_maps = [core_inputs(I, c, shared) for c in range(8)]
    nc = build_nc()
    res = run_bass_kernel_spmd(nc, in_maps, core_ids=list(range(8)))
    y_prompt = np.zeros((16, 256, D), np.float32)
    y_sample = np.zeros((2, 4096, D), np.float32)
    nck = np.zeros((16, 1, 256, 2, 128), np.float32)
    ncv = np.zeros((16, 1, 256, 2, 128), np.float32)
    for c in range(8):
        r = res.results[c]
        sb, qt = c // 4, c % 4
        y = r["y_own"]
        y_prompt[2 * c] = y[0:256]
        y_prompt[2 * c + 1] = y[256:512]
        y_sample[sb, qt * 1024:(qt + 1) * 1024] = y[512:]
        nck[2 * c, 0] = r["nk"][0:256].reshape(256, 2, 128)
        nck[2 * c + 1, 0] = r["nk"][256:512].reshape(256, 2, 128)
        ncv[2 * c, 0] = r["nv"][0:256].reshape(256, 2, 128)
        ncv[2 * c + 1, 0] = r["nv"][256:512].reshape(256, 2, 128)
    return (y_prompt, y_sample, nck, ncv)
```

```python
import contextlib
import os
import numpy as np
import concourse.bass as bass
import concourse.mybir as mybir
from concourse.bass_utils import run_bass_kernel_spmd

F32 = mybir.dt.float32
BF16 = mybir.dt.bfloat16
I32 = mybir.dt.int32
U32 = mybir.dt.uint32
ALU = mybir.AluOpType
AF = mybir.ActivationFunctionType
AX = mybir.AxisListType

D = 2048
NE = 32
CAP = 512
NTOK = 1536
NT = 12
EPS = 1e-6
SAME_ENG_SYNC = True
UPTO = 99
DEBUG = False
NOSC = False


def _region(ap):
    name = ap.tensor.name
    pat = ap.ap
    off = int(ap.offset)
    es = mybir.dt.size(ap.dtype)
    sp = str(ap.space)
    if sp in ("SB", "PSUM"):
        pstride, npart = pat[0]
        if pstride == 0:
            pstride = 1 << 40
        p0 = off // pstride
        f0 = off % pstride
        ext = 1 + sum((cnt - 1) * abs(step) for step, cnt in pat[1:])
        if sp == "PSUM":
            b0 = (f0 * es) // 2048 * 2048
            b1 = ((f0 + ext) * es + 2047) // 2048 * 2048
            return (name, 0, 128, b0, b1)
        return (name, p0, p0 + npart, f0 * es, (f0 + ext) * es)
    ext = 1 + sum((cnt - 1) * abs(step) for step, cnt in pat)
    return (name, 0, 1, off * es, (off + ext) * es)


class _Op:
    __slots__ = ("eng", "fn", "is_dma", "dkey", "waits", "signal", "sigidx", "dcount", "pos", "deps", "dslot")


class Sched:
    ENGS = ("pe", "act", "dve", "pool", "sp")
    NDSEM = 20

    def __init__(self, nc):
        self.nc = nc
        self.ops = []
        self.acc = {}
        self.dcount = {}
        self.notrack = set()

    def add(self, eng, fn, reads=(), writes=(), dkey=None):
        op = _Op()
        op.eng = eng
        op.fn = fn
        op.is_dma = dkey is not None
        op.dkey = dkey
        op.signal = False
        op.sigidx = 0
        op.waits = []
        op.deps = set()
        idx = len(self.ops)
        if op.is_dma:
            n = self.dcount.get(eng, 0)
            self.dcount[eng] = n + 1
            op.dslot = (eng, n % self.NDSEM)
            op.dcount = n // self.NDSEM + 1
        for is_w, aps in ((False, reads), (True, writes)):
            for ap in aps:
                r = ap if isinstance(ap, tuple) else _region(ap)
                name = r[0]
                if name in self.notrack:
                    continue
                lst = self.acc.get(name, [])
                keep = []
                for a in lst:
                    ov = a[2] < r[2] and r[1] < a[3] and a[4] < r[4] and r[3] < a[5]
                    if ov and (a[1] or is_w) and a[0] != idx:
                        op.deps.add(a[0])
                    if is_w and ov and a[0] != idx and r[1] <= a[2] and a[3] <= r[2] and r[3] <= a[4] and a[5] <= r[4]:
                        continue
                    keep.append(a)
                keep.append([idx, is_w, r[1], r[2], r[3], r[4]])
                self.acc[name] = keep
        self.ops.append(op)
        return idx

    def finish(self, final_eng="sp"):
        nc = self.nc
        ops = self.ops
        cnt = {e: 0 for e in self.ENGS}
        for op in ops:
            cnt[op.eng] += 1
            op.pos = cnt[op.eng]
        waited = {e: {} for e in self.ENGS}
        edges = []
        prev_on_slot = {}
        for i, op in enumerate(ops):
            E = op.eng
            if op.is_dma:
                if op.dslot in prev_on_slot:
                    op.deps.add(prev_on_slot[op.dslot])
                prev_on_slot[op.dslot] = i
            for j in sorted(op.deps):
                p = ops[j]
                if p.is_dma:
                    key = ("d", p.dslot)
                    val = p.dcount
                else:
                    if p.eng == E and (E == "pe" or not SAME_ENG_SYNC):
                        continue
                    key = ("e", p.eng)
                    val = p.pos
                if waited[E].get(key, 0) >= val:
                    continue
                waited[E][key] = val
                if not p.is_dma:
                    p.signal = True
                edges.append((i, key, j))
        sc = {e: 0 for e in self.ENGS}
        for op in ops:
            if op.signal and not op.is_dma:
                sc[op.eng] += 1
                op.sigidx = sc[op.eng]
        stack = contextlib.ExitStack()
        esem = {e: stack.enter_context(nc.semaphore("es_" + e)) for e in self.ENGS}
        slots = {}
        for op in ops:
            if op.is_dma:
                slots[op.dslot] = max(slots.get(op.dslot, 0), op.dcount)
        dsem = {k: stack.enter_context(nc.semaphore("ds_%s_%d" % k)) for k in slots}
        for (i, key, j) in edges:
            p = ops[j]
            if key[0] == "d":
                ops[i].waits.append((dsem[p.dslot], 16 * p.dcount))
            else:
                ops[i].waits.append((esem[p.eng], p.sigidx))
        for op in ops:
            if op.is_dma and op.dcount > 1 and waited[op.eng].get(("d", op.dslot), 0) < op.dcount - 1:
                pass
        final_waits = [(dsem[k], 16 * c) for k, c in slots.items()]
        by_eng = {e: [op for op in ops if op.eng == e] for e in self.ENGS}
        self.stats = {e: len(by_eng[e]) for e in self.ENGS}
        self.stats["edges"] = len(edges)
        self.stats["dsems"] = len(dsem)

        def replay(ename, e):
            for op in by_eng[ename]:
                for (s, v) in op.waits:
                    e.wait_ge(s, v)
                ins = op.fn(e)
                if op.is_dma:
                    ins.then_inc(dsem[op.dslot], 16)
                elif op.signal:
                    ins.then_inc(esem[ename], 1)
            if ename == final_eng:
                for (s, v) in final_waits:
                    e.wait_ge(s, v)

        with nc.Block() as block:
            @block.tensor
            def _(e):
                replay("pe", e)

            @block.scalar
            def _(e):
                replay("act", e)

            @block.vector
            def _(e):
                replay("dve", e)

            @block.gpsimd
            def _(e):
                replay("pool", e)

            @block.sync
            def _(e):
                replay("sp", e)
        stack.close()


class Arena:
    def __init__(self, base, nbytes):
        self.base = base
        self.free = [(0, nbytes)]
        self.live = {}
        self.peak = 0

    def alloc(self, shape, dtype=F32):
        es = mybir.dt.size(dtype)
        n = 1
        for s in shape[1:]:
            n *= s
        nb = (n * es + 63) // 64 * 64
        cur = getattr(self, "cursor", 0)
        cand = None
        for (o_, l_) in self.free:
            if o_ + l_ - max(o_, cur) >= nb and o_ + l_ > cur:
                cand = max(o_, cur)
                break
        if cand is None:
            for (o_, l_) in self.free:
                if l_ >= nb:
                    cand = o_
                    break
        if cand is None:
            raise RuntimeError("arena full: want %d, free %s" % (nb, self.free))
        o = cand
        newfree = []
        for (o_, l_) in self.free:
            if o_ <= o and o + nb <= o_ + l_:
                if o > o_:
                    newfree.append((o_, o - o_))
                if o_ + l_ > o + nb:
                    newfree.append((o + nb, o_ + l_ - (o + nb)))
            else:
                newfree.append((o_, l_))
        self.free = newfree
        self.cursor = o + nb
        self.peak = max(self.peak, o + nb)
        v = self.base[0:shape[0], o // 4:(o + nb) // 4]
        if dtype != F32:
            v = v.bitcast(dtype)
        v = v[:, 0:n]
        if len(shape) == 3:
            v = v.rearrange("p (a b) -> p a b", a=shape[1])
        elif len(shape) == 4:
            v = v.rearrange("p (a b c) -> p a b c", a=shape[1], b=shape[2])
        self.live[id(v)] = (o, nb, v)
        return v

    def release(self, v):
        o, nb, _ = self.live.pop(id(v))
        self.free.append((o, nb))
        self.free.sort()
        merged = []
        for (a, l) in self.free:
            if merged and merged[-1][0] + merged[-1][1] == a:
                merged[-1] = (merged[-1][0], merged[-1][1] + l)
            else:
                merged.append((a, l))
        self.free = merged


def build_nc(debug=False, upto=99):
    nc = bass.Bass("TRN2", target_bir_lowering=False)

    def din(name, shape, dt=F32):
        return nc.dram_tensor(name, shape, dt, kind="ExternalInput").ap()

    def dout(name, shape, dt=F32):
        return nc.dram_tensor(name, shape, dt, kind="ExternalOutput").ap()

    x_own = din("x_own", [NTOK, D])
    x_kv = din("x_kv", [4096, D])
    ck = din("ck", [256, 256])
    cv = din("cv", [256, 256])
    cT = din("cT", [128, 16, 2])
    w_mod = din("w_mod", [D, 6 * D])
    b_mod = din("b_mod", [1, 6 * D])
    norm1 = din("norm1", [1, D])
    norm2 = din("norm2", [1, D])
    norm_f = din("norm_f", [1, D])
    w_in = din("w_in", [D, 3584])
    w_out = din("w_out", [D, D])
    w_router = din("w_router", [D, NE])
    b_router = din("b_router", [1, NE])
    q_gain = din("q_gain", [1, 128])
    k_gain = din("k_gain", [1, 128])
    w_spT = din("w_spT", [128, 8, 128])
    b_spT = din("b_spT", [128, 8])
    ag = din("ag", [1, 1024])
    gg = din("gg", [1, 1024])
    w_gate = din("w_gate", [NE, D, D])
    w_up = din("w_up", [NE, D, D])
    w_down = din("w_down", [NE, D, D])
    bgT = din("bgT", [128, NE, 16])
    buT = din("buT", [128, NE, 16])
    b_down = din("b_down", [NE, D])
    cos_q = din("cos_q", [1024, 128])
    sin_q = din("sin_q", [1024, 128])
    cos_k = din("cos_k", [4096, 128])
    sin_k = din("sin_k", [4096, 128])

    y_own = dout("y_own", [NTOK, D])
    nk = dout("nk", [512, 256])
    nv = dout("nv", [512, 256])
    dbg = {}
    if debug:
        dbg["x1"] = dout("dbg_x1", [NTOK, D])
        dbg["h2"] = dout("dbg_h2", [NTOK, D])
        dbg["lg"] = dout("dbg_lg", [NTOK, NE])
        dbg["idx"] = dout("dbg_idx", [NTOK, 8], I32)
        dbg["gate"] = dout("dbg_gate", [NTOK, 4])
        dbg["mods"] = dout("dbg_mods", [2, 6, D])
        dbg["f"] = dout("dbg_f", [NTOK, D])

    MODS = nc.dram_tensor("MODS", [2, 6, D], F32, kind="Internal").ap()
    X1 = nc.dram_tensor("X1", [NTOK, D], F32, kind="Internal").ap()
    XS = nc.dram_tensor("XS", [NE * CAP + 128, D], BF16, kind="Internal").ap()
    YB = nc.dram_tensor("YB", [NE * CAP + 128, D], F32, kind="Internal").ap()
    ZROW = NE * CAP

    es = contextlib.ExitStack()
    SBN = 200 * 1024
    SBT = es.enter_context(nc.sbuf_tensor("SB", [128, SBN // 4], F32))
    PS = es.enter_context(nc.psum_tensor("PS", [128, 8, 512], F32))
    A = Arena(SBT[:, :], SBN)
    S = Sched(nc)
    S.notrack.update(["x_own", "x_kv", "ck", "cv", "cT", "w_mod", "b_mod", "norm1", "norm2", "norm_f", "w_in", "w_out",
                      "w_router", "b_router", "q_gain", "k_gain", "w_spT", "b_spT", "ag", "gg", "w_gate", "w_up", "w_down",
                      "bgT", "buT", "b_down", "cos_q", "sin_q", "cos_k", "sin_k"])

    dump_i = [0]
    dump_flag = [0]

    def dump(name, ap, dt=F32):
        if not debug:
            return
        o = nc.dram_tensor("dmp_" + name, list(ap.shape), dt, kind="ExternalOutput").ap()
        dump_i[0] += 1
        S.add("sp", lambda e: e.dma_start(out=o, in_=ap), reads=[ap], writes=[o], dkey="dump%d" % dump_i[0])

    def aps(*xs):
        return [x for x in xs if x is not None and not isinstance(x, (int, float))]

    def dma(eng, out, in_, key):
        S.add(eng, lambda e: e.dma_start(out=out, in_=in_), reads=[in_], writes=[out], dkey=key)

    def mm(out, lhsT, rhs, start=True, stop=True):
        S.add("pe", lambda e: e.matmul(out, lhsT=lhsT, rhs=rhs, start=start, stop=stop), reads=[lhsT, rhs], writes=[out])

    def act(out, in_, func, scale=1.0, bias=0.0, accum=None):
        kw = {}
        if accum is not None:
            kw["accum_out"] = accum
        S.add("act", lambda e: e.activation(out=out, in_=in_, func=func, scale=scale, bias=bias, **kw),
              reads=aps(in_, scale, bias), writes=aps(out, accum))

    def ts(eng, out, in0, s1, s2, op0, op1=None, accum=None):
        kw = {}
        if op1 is not None:
            kw["op1"] = op1
        if accum is not None:
            kw["accum_out"] = accum
        S.add(eng, lambda e: e.tensor_scalar(out=out, in0=in0, scalar1=s1, scalar2=s2, op0=op0, **kw),
              reads=aps(in0, s1, s2), writes=aps(out, accum))

    def tt(eng, out, in0, in1, op):
        S.add(eng, lambda e: e.tensor_tensor(out=out, in0=in0, in1=in1, op=op), reads=[in0, in1], writes=[out])

    def stt(eng, out, in0, scalar, in1, op0, op1):
        S.add(eng, lambda e: e.scalar_tensor_tensor(out=out, in0=in0, scalar=scalar, in1=in1, op0=op0, op1=op1),
              reads=aps(in0, scalar, in1), writes=[out])

    def cp(eng, out, in_):
        if eng == "act":
            S.add("act", lambda e: e.copy(out=out, in_=in_), reads=[in_], writes=[out])
        else:
            S.add(eng, lambda e: e.tensor_copy(out=out, in_=in_), reads=[in_], writes=[out])

    def recip(out, in_):
        S.add("dve", lambda e: e.reciprocal(out=out, in_=in_), reads=[in_], writes=[out])

    def memset(eng, out, val):
        S.add(eng, lambda e: e.memset(out, val), writes=[out])

    iof = A.alloc([128, 128])
    iop = A.alloc([128, 1])
    ident = A.alloc([128, 128])
    tri_b = A.alloc([128, 128], BF16)
    ones_b = A.alloc([128, 128], BF16)
    ones_f = A.alloc([128, 128])
    epsc = A.alloc([128, 1])
    S.add("pool", lambda e: e.iota(iof, pattern=[[1, 128]], base=0, channel_multiplier=0, allow_small_or_imprecise_dtypes=True), writes=[iof])
    S.add("pool", lambda e: e.iota(iop, pattern=[[0, 1]], base=0, channel_multiplier=1, allow_small_or_imprecise_dtypes=True), writes=[iop])
    ts("dve", ident, iof, iop[:, 0:1], None, ALU.is_equal)
    ts("dve", tri_b, iof, iop[:, 0:1], None, ALU.is_gt)
    memset("dve", ones_b, 1.0)
    memset("dve", ones_f, 1.0)
    memset("dve", epsc, EPS)
    c7 = A.alloc([128, 1])
    cm6 = A.alloc([128, 1])
    memset("dve", c7, 7.0)
    memset("dve", cm6, -6.0)
    tz = A.alloc([128, 1])
    ts("dve", tz, iop, float(NE * CAP), None, ALU.add)

    def tr(out, in_):
        k = in_.shape[0]
        S.add("pe", lambda e: e.transpose(out=out, in_=in_, identity=ident[0:k, 0:k]), reads=[in_, ident[0:k, 0:k]], writes=[out])

    evac_rr = [0]

    def evac(out, in_):
        evac_rr[0] += 1
        cp("act" if evac_rr[0] % 2 else "dve", out, in_)

    def rstd_from_ss(rs, ss, n):
        act(rs, ss, AF.Sqrt, scale=1.0 / n, bias=epsc[:, 0:1])
        recip(rs, rs)

    def bc_load(dst, src_row, key="bc"):
        dma("sp", dst, src_row.to_broadcast([128, src_row.shape[-1]]), key)

    def emit():
        wring = [A.alloc([128, 16, 512], BF16) for _ in range(3)]
        wr_i = [0]

        def wload(src2d):
            slot = wr_i[0] % len(wring)
            wr_i[0] += 1
            w = wring[slot]
            dma("pool", w, src2d.rearrange("(kc p) n -> p kc n", p=128), "w%d" % slot)
            return w

        zb = A.alloc([128, 8192], BF16)
        memset("dve", zb, 0.0)
        for i in range(NE):
            dma("sp", XS[i * CAP:(i + 1) * CAP, :].rearrange("(p t) d -> p (t d)", p=128), zb, "xsz")
        dma("sp", XS[NE * CAP:NE * CAP + 128, :], zb[:, 0:D], "xsz")
        cT_sb = A.alloc([128, 16, 2])
        sc_b = A.alloc([128, 16, 2], BF16)
        dma("sp", cT_sb, cT, "misc")
        act(sc_b, cT_sb, AF.Silu)
        nrm = A.alloc([2, 2, D])
        for j in range(2):
            dma("sp", nrm[j:j + 1, 0, :], norm1, "misc")
            dma("sp", nrm[j:j + 1, 1, :], norm2, "misc")
        bm = [A.alloc([1, 512]) for _ in range(2)]
        mrow = [A.alloc([2, 512]) for _ in range(2)]
        for pc in range(24):
            comp, cb = pc // 4, (pc % 4) * 512
            w = wload(w_mod[:, pc * 512:(pc + 1) * 512])
            bmt = bm[pc % 2]
            dma("sp", bmt, b_mod[0:1, pc * 512:(pc + 1) * 512], "bm%d" % (pc % 2))
            pb = PS[0:2, pc % 2, :]
            for kc in range(16):
                mm(pb, sc_b[:, kc, :], w[:, kc, :], start=(kc == 0), stop=False)
            mm(pb, ones_f[0:1, 0:2], bmt, start=False, stop=True)
            mr = mrow[pc % 2]
            if comp in (1, 4):
                stt("dve", mr, pb, 1.0, nrm[:, 0 if comp == 1 else 1, cb:cb + 512], ALU.add, ALU.mult)
            else:
                cp("dve", mr, pb)
            dma("sp", MODS[:, comp, cb:cb + 512], mr, "mods")
        if debug:
            dtmp = A.alloc([2, 6, D])
            dma("sp", dtmp, MODS, "dbgl")
            dma("sp", dbg["mods"], dtmp, "dbg")
            A.release(dtmp)
        for t_ in (zb, cT_sb, sc_b, nrm, bm[0], bm[1], mrow[0], mrow[1]):
            A.release(t_)

        psr = [0]

        def bank(i=None):
            if i is None:
                psr[0] += 1
                i = psr[0] % 8
            return PS[:, i, :]

        def make_h(src_rows, Abc, Bbc, hT_dst, col0):
            xt = A.alloc([128, D])
            hb = A.alloc([128, D])
            ss = A.alloc([128, 2])
            dma("sp", xt, src_rows, "xt")
            act(hb, xt, AF.Square, accum=ss[:, 0:1])
            rstd_from_ss(ss[:, 1:2], ss[:, 0:1], D)
            act(hb, xt, AF.Identity, scale=ss[:, 1:2])
            tt("dve", hb, hb, Abc, ALU.mult)
            tt("pool", hb, hb, Bbc, ALU.add)
            if dump_flag[0] == 1:
                dump("hb", hb)
                dump("xt", xt)
                dump("ss", ss)
            for g4 in range(4):
                pb = bank()
                for j in range(4):
                    kc = g4 * 4 + j
                    tr(pb[:, j * 128:(j + 1) * 128], hb[:, kc * 128:(kc + 1) * 128])
                evac(hT_dst[:, g4 * 4:(g4 + 1) * 4, col0:col0 + 128], pb.rearrange("p (a b) -> p a b", a=4))
            A.release(xt)
            A.release(hb)
            A.release(ss)

        def heads_post(pb, nh, gain_bc, cosT, sinT, dstT_list, f32_out=None):
            ss = A.alloc([128, 2 * nh])
            junk = A.alloc([128, 128])
            for h in range(nh):
                act(junk, pb[:, h * 128:(h + 1) * 128], AF.Square, accum=ss[:, h:h + 1])
            rstd_from_ss(ss[:, nh:2 * nh], ss[:, 0:nh], 128)
            for h in range(nh):
                kn = A.alloc([128, 128]) if f32_out is None else f32_out[h]
                stt("dve", kn, pb[:, h * 128:(h + 1) * 128], ss[:, nh + h:nh + h + 1], gain_bc, ALU.mult, ALU.mult)
                if cosT is not None:
                    t1 = A.alloc([128, 128])
                    t2 = A.alloc([128, 128])
                    tt("dve", t1, kn, cosT, ALU.mult)
                    kn4 = kn.rearrange("p (a b c) -> p a b c", a=2, b=2)
                    t24 = t2.rearrange("p (a b c) -> p a b c", a=2, b=2)
                    sn4 = sinT.rearrange("p (a b c) -> p a b c", a=2, b=2)
                    tt("pool", t24[:, :, 0, :], kn4[:, :, 1, :], sn4[:, :, 0, :], ALU.mult)
                    tt("pool", t24[:, :, 1, :], kn4[:, :, 0, :], sn4[:, :, 1, :], ALU.mult)
                    tt("dve", t1, t1, t2, ALU.add)
                    src = t1
                else:
                    src = kn
                pt = bank()
                tr(pt[:, 0:128], src)
                evac(dstT_list[h], pt[:, 0:128])
                if cosT is not None:
                    A.release(t1)
                    A.release(t2)
                if f32_out is None:
                    A.release(kn)
            A.release(ss)
            A.release(junk)

        if upto < 1:
            return
        kT_all = A.alloc([128, 2, 4352], BF16)
        V_all = A.alloc([128, 34, 2, 132], BF16)
        memset("pool", V_all, 1.0)
        kg_bc = A.alloc([128, 128])
        qg_bc = A.alloc([128, 128])
        bc_load(kg_bc, k_gain[0:1, :])
        bc_load(qg_bc, q_gain[0:1, :])
        ckt = A.alloc([128, 2, 256])
        dma("sp", ckt, ck.rearrange("(t p) f -> p t f", p=128), "misc")
        cvt = A.alloc([128, 2, 256])
        dma("sp", cvt, cv.rearrange("(t p) f -> p t f", p=128), "misc")
        for t in range(2):
            cp("dve", V_all[:, t, :, 0:128], cvt[:, t, :].rearrange("p (h d) -> p h d", h=2))
        A.release(cvt)
        for t in range(2):
            for h in range(2):
                pt = bank()
                tr(pt[:, 0:128], ckt[:, t, h * 128:(h + 1) * 128])
                evac(kT_all[:, h, t * 128:(t + 1) * 128], pt[:, 0:128])
        A.release(ckt)

        A1s = A.alloc([128, D])
        B1s = A.alloc([128, D])
        bc_load(A1s, MODS[1:2, 1, :])
        bc_load(B1s, MODS[1:2, 0, :])
        w_kv = wload(w_in[:, 1024:1536])
        hTk = [A.alloc([128, 16, 128], BF16) for _ in range(2)]
        for i in range(32):
            hT = hTk[i % 2]
            make_h(x_kv[i * 128:(i + 1) * 128, :], A1s, B1s, hT, 0)
            pb = bank()
            for kc in range(16):
                mm(pb, hT[:, kc, :], w_kv[:, kc, :], start=(kc == 0), stop=(kc == 15))
            cosT = A.alloc([128, 128])
            sinT = A.alloc([128, 128])
            dma("sp", cosT, cos_k[i * 128:(i + 1) * 128, :], "rope")
            dma("sp", sinT, sin_k[i * 128:(i + 1) * 128, :], "rope")
            heads_post(pb, 2, kg_bc, cosT, sinT, [kT_all[:, h, 256 + i * 128:256 + (i + 1) * 128] for h in range(2)])
            evac(V_all[:, 2 + i, :, 0:128], pb[:, 256:512].rearrange("p (h d) -> p h d", h=2))
            A.release(cosT)
            A.release(sinT)
        for t_ in hTk:
            A.release(t_)

        if upto < 2:
            return
        A.release(A1s)
        A.release(B1s)
        ag_bc = A.alloc([128, 1024])
        gg_bc = A.alloc([128, 1024])
        bc_load(ag_bc, ag[0:1, :])
        bc_load(gg_bc, gg[0:1, :])
        wsp_b = A.alloc([128, 8, 128], BF16)
        dma("pool", wsp_b, w_spT, "misc2")
        bsp = A.alloc([128, 8])
        dma("sp", bsp, b_spT, "misc")
        wr_sb = A.alloc([128, 16, NE])
        dma("sp", wr_sb, w_router.rearrange("(kc p) n -> p kc n", p=128), "misc")
        br_sb = A.alloc([1, NE])
        dma("sp", br_sb, b_router, "misc")
        maskb = A.alloc([128, NT, NE], BF16)
        idx_all = A.alloc([128, NT, 8], I32)
        gate_all = A.alloc([128, NT, 4])
        gmT_all = A.alloc([NE, NT, 128])
        scale_att = 128.0 ** -0.5
        if 2 <= upto <= 2.01:
            return

        for blk in range(6):
            is_p = blk < 2
            g = 0 if is_p else 1
            Abc = A.alloc([128, D])
            Bbc = A.alloc([128, D])
            bc_load(Abc, MODS[g:g + 1, 1, :])
            bc_load(Bbc, MODS[g:g + 1, 0, :])
            tiles = [2 * blk, 2 * blk + 1]
            hTb = A.alloc([128, 16, 256], BF16)
            for j, t in enumerate(tiles):
                dump_flag[0] = 1 if t == 0 else 0
                make_h(x_own[t * 128:(t + 1) * 128, :], Abc, Bbc, hTb, j * 128)
            dump_flag[0] = 0
            if blk == 0:
                dump("hTb", hTb, BF16)
            A.release(Abc)
            A.release(Bbc)
            if 2 <= upto <= 2.02:
                return
            qTb = A.alloc([128, 8, 256], BF16)
            if is_p:
                kTp = A.alloc([128, 2, 256], BF16)
                Vp = A.alloc([128, 2, 2, 132], BF16)
                memset("pool", Vp, 1.0)
            for pi in range(3 if is_p else 2):
                if 2 <= upto <= 2.03 + 0.01 * pi - 0.001:
                    return
                w = wload(w_in[:, pi * 512:(pi + 1) * 512])
                for j, t in enumerate(tiles):
                    pb = bank()
                    for kc in range(16):
                        mm(pb, hTb[:, kc, j * 128:(j + 1) * 128], w[:, kc, :], start=(kc == 0), stop=(kc == 15))
                    if pi < 2:
                        if is_p:
                            cosT = sinT = None
                        else:
                            cosT = A.alloc([128, 128])
                            sinT = A.alloc([128, 128])
                            r0 = (t - 4) * 128
                            dma("sp", cosT, cos_q[r0:r0 + 128, :], "rope")
                            dma("sp", sinT, sin_q[r0:r0 + 128, :], "rope")
                        heads_post(pb, 4, qg_bc, cosT, sinT, [qTb[:, pi * 4 + h, j * 128:(j + 1) * 128] for h in range(4)])
                        if cosT is not None:
                            A.release(cosT)
                            A.release(sinT)
                    else:
                        kfs = [A.alloc([128, 128]) for _ in range(2)]
                        heads_post(pb, 2, kg_bc, None, None, [kTp[:, h, j * 128:(j + 1) * 128] for h in range(2)], f32_out=kfs)
                        for h in range(2):
                            dma("sp", nk[t * 128:(t + 1) * 128, h * 128:(h + 1) * 128], kfs[h], "nk")
                        vf = A.alloc([128, 256])
                        cp("act", vf, pb[:, 256:512])
                        dma("sp", nv[t * 128:(t + 1) * 128, :], vf, "nv")
                        evac(Vp[:, j, :, 0:128], pb[:, 256:512].rearrange("p (h d) -> p h d", h=2))
                        A.release(kfs[0])
                        A.release(kfs[1])
                        A.release(vf)
            if 2 <= upto <= 2.1:
                return
            at_tok = A.alloc([128, 2, 1024])
            nkt = 2 if is_p else 34
            pTr = [A.alloc([128, 256], BF16) for _ in range(3)]
            for h in range(8):
                hk = h // 4
                pob = 2 + 2 * (h % 2)

                def kt_ap(kt):
                    return (kTp[:, hk, kt * 128:(kt + 1) * 128] if is_p else kT_all[:, hk, kt * 128:(kt + 1) * 128])

                def v_ap(kt):
                    return (Vp[:, kt, hk, 0:129] if is_p else V_all[:, kt, hk, 0:129])

                def score(kt):
                    mm(PS[:, 6 + (kt % 2), 0:256], kt_ap(kt), qTb[:, h, :])

                score(0)
                if nkt > 1:
                    score(1)
                for kt in range(nkt):
                    pT = pTr[kt % 3]
                    act(pT, PS[:, 6 + (kt % 2), 0:256], AF.Exp, scale=scale_att)
                    for j in range(2):
                        mm(PS[:, pob + j, 0:129], pT[:, j * 128:(j + 1) * 128], v_ap(kt), start=(kt == 0), stop=(kt == nkt - 1))
                    if kt + 2 < nkt:
                        score(kt + 2)
                rs2 = A.alloc([128, 2])
                for j in range(2):
                    recip(rs2[:, j:j + 1], PS[:, pob + j, 128:129])
                    ts("dve", at_tok[:, j, h * 128:(h + 1) * 128], PS[:, pob + j, 0:128], rs2[:, j:j + 1], None, ALU.mult)
                A.release(rs2)
            for t_ in pTr:
                A.release(t_)
            A.release(qTb)
            if is_p:
                A.release(kTp)
                A.release(Vp)
            if 2 <= upto <= 2.2:
                return
            gm_tok = A.alloc([128, 2, 1024])
            for half in range(2):
                wg_ = wload(w_in[:, 2560 + half * 512:2560 + (half + 1) * 512])
                wu_ = wload(w_in[:, 1536 + half * 512:1536 + (half + 1) * 512])
                for j, t in enumerate(tiles):
                    pg = bank()
                    pu = bank()
                    for kc in range(16):
                        mm(pg, hTb[:, kc, j * 128:(j + 1) * 128], wg_[:, kc, :], start=(kc == 0), stop=(kc == 15))
                    for kc in range(16):
                        mm(pu, hTb[:, kc, j * 128:(j + 1) * 128], wu_[:, kc, :], start=(kc == 0), stop=(kc == 15))
                    ggl = A.alloc([128, 512])
                    ugl = A.alloc([128, 512])
                    gcb = A.alloc([128, 4, 128], BF16)
                    ss = A.alloc([128, 8])
                    junk = A.alloc([128, 128])
                    act(ggl, pg, AF.Gelu_apprx_tanh)
                    act(ugl, pu, AF.Gelu_apprx_tanh)
                    for hh in range(4):
                        act(junk, ggl[:, hh * 128:(hh + 1) * 128], AF.Square, accum=ss[:, hh:hh + 1])
                    rstd_from_ss(ss[:, 4:8], ss[:, 0:4], 128)
                    for hh in range(4):
                        ts("dve", gcb[:, hh, :], ggl[:, hh * 128:(hh + 1) * 128], ss[:, 4 + hh:5 + hh], None, ALU.mult)
                    psp = bank()
                    for hh in range(4):
                        mm(psp[:, hh * 128:(hh + 1) * 128], wsp_b[:, half * 4 + hh, :], gcb[:, hh, :])
                    for hh in range(4):
                        hd = half * 4 + hh
                        stt("dve", gm_tok[:, j, hd * 128:(hd + 1) * 128], psp[:, hh * 128:(hh + 1) * 128], bsp[:, hd:hd + 1],
                            ugl[:, hh * 128:(hh + 1) * 128], ALU.add, ALU.mult)
                    for t_ in (ggl, ugl, gcb, ss, junk):
                        A.release(t_)
            A.release(hTb)
            if 2 <= upto <= 2.3:
                return
            mTb = A.alloc([128, 16, 256], BF16)
            for j in range(2):
                for which, src, gbc in ((0, at_tok, ag_bc), (1, gm_tok, gg_bc)):
                    ss = A.alloc([128, 2])
                    mg = A.alloc([128, 1024])
                    act(mg, src[:, j, :], AF.Square, accum=ss[:, 0:1])
                    rstd_from_ss(ss[:, 1:2], ss[:, 0:1], 1024)
                    stt("dve", mg, src[:, j, :], ss[:, 1:2], gbc, ALU.mult, ALU.mult)
                    for g4 in range(2):
                        pb = bank()
                        for jj in range(4):
                            c0 = (g4 * 4 + jj) * 128
                            tr(pb[:, jj * 128:(jj + 1) * 128], mg[:, c0:c0 + 128])
                        evac(mTb[:, which * 8 + g4 * 4:which * 8 + (g4 + 1) * 4, j * 128:(j + 1) * 128], pb.rearrange("p (a b) -> p a b", a=4))
                    A.release(ss)
                    A.release(mg)
            A.release(at_tok)
            A.release(gm_tok)
            if 2 <= upto <= 2.4:
                return
            g1bc = A.alloc([128, D])
            A2bc = A.alloc([128, D])
            B2bc = A.alloc([128, D])
            bc_load(g1bc, MODS[g:g + 1, 2, :])
            bc_load(A2bc, MODS[g:g + 1, 4, :])
            bc_load(B2bc, MODS[g:g + 1, 3, :])
            for n in range(4):
                w = wload(w_out[:, n * 512:(n + 1) * 512])
                for j in range(2):
                    pb = PS[:, j * 4 + n, :]
                    for kc in range(16):
                        mm(pb, mTb[:, kc, j * 128:(j + 1) * 128], w[:, kc, :], start=(kc == 0), stop=(kc == 15))
            for j, t in enumerate(tiles):
                lb_i = [0]

                def lbank(j=j, lb_i=lb_i):
                    lb_i[0] += 1
                    return PS[:, j * 4 + (lb_i[0] % 4), :]

                xt = A.alloc([128, D])
                x1 = A.alloc([128, D])
                dma("sp", xt, x_own[t * 128:(t + 1) * 128, :], "xt2")
                for n in range(4):
                    tt("dve", x1[:, n * 512:(n + 1) * 512], PS[:, j * 4 + n, :], g1bc[:, n * 512:(n + 1) * 512], ALU.mult)
                tt("pool", x1, x1, xt, ALU.add)
                dma("sp", X1[t * 128:(t + 1) * 128, :], x1, "x1w")
                if debug:
                    dma("sp", dbg["x1"][t * 128:(t + 1) * 128, :], x1, "dbg")
                if 2 <= upto <= 2.5:
                    return
                ss = A.alloc([128, 2])
                h2 = xt
                act(h2, x1, AF.Square, accum=ss[:, 0:1])
                rstd_from_ss(ss[:, 1:2], ss[:, 0:1], D)
                act(h2, x1, AF.Identity, scale=ss[:, 1:2])
                tt("dve", h2, h2, A2bc, ALU.mult)
                tt("pool", h2, h2, B2bc, ALU.add)
                if debug:
                    dma("sp", dbg["h2"][t * 128:(t + 1) * 128, :], h2, "dbg")
                h2b = A.alloc([128, D], BF16)
                cp("act", h2b, h2)
                h2T = A.alloc([128, 16, 128])
                for g4 in range(4):
                    pb = lbank()
                    for jj in range(4):
                        kc = g4 * 4 + jj
                        tr(pb[:, jj * 128:(jj + 1) * 128], h2[:, kc * 128:(kc + 1) * 128])
                    evac(h2T[:, g4 * 4:(g4 + 1) * 4, :], pb.rearrange("p (a b) -> p a b", a=4))
                if 2 <= upto <= 2.6:
                    return
                pl = lbank()
                for kc in range(16):
                    mm(pl[:, 0:NE], h2T[:, kc, :], wr_sb[:, kc, :], start=(kc == 0), stop=False)
                mm(pl[:, 0:NE], ones_f[0:1, 0:128], br_sb, start=False, stop=True)
                lg = A.alloc([128, NE])
                cp("dve", lg, pl[:, 0:NE])
                if debug:
                    dma("sp", dbg["lg"][t * 128:(t + 1) * 128, :], lg, "dbg")
                if 2 <= upto <= 2.7:
                    return
                mx = A.alloc([128, 8])
                mi = A.alloc([128, 8], U32)
                ef = A.alloc([128, 8])
                S.add("dve", lambda e, mx=mx, lg=lg: e.max(out=mx, in_=lg), reads=[lg], writes=[mx])
                S.add("dve", lambda e, mx=mx, lg=lg, mi=mi: e.max_index(out=mi, in_max=mx, in_values=lg), reads=[lg, mx], writes=[mi])
                cp("dve", ef, mi)
                ts("dve", maskb[:, t, :], lg, mx[:, 3:4], None, ALU.is_ge)
                nmx = A.alloc([128, 2])
                ts("dve", nmx[:, 0:1], mx[:, 0:1], -1.0, None, ALU.mult)
                ex4 = A.alloc([128, 4])
                act(ex4, mx[:, 0:4], AF.Exp, bias=nmx[:, 0:1], accum=nmx[:, 1:2])
                recip(nmx[:, 1:2], nmx[:, 1:2])
                if 2 <= upto <= 2.8:
                    return
                pp = lbank()
                for i2 in range(t):
                    mm(pp[:, 0:NE], ones_b, maskb[:, i2, :], start=(i2 == 0), stop=False)
                mm(pp[:, 0:NE], tri_b, maskb[:, t, :], start=(t == 0), stop=True)
                posf = A.alloc([128, NE])
                cp("dve", posf, pp[:, 0:NE])
                gm_d = A.alloc([128, NE])
                idxf = A.alloc([128, 8])
                oh = A.alloc([128, NE])
                pr = A.alloc([128, NE])
                pk = A.alloc([128, 4])
                vk = A.alloc([128, 4])
                for k in range(4):
                    ts("dve", oh, iof[:, 0:NE], ef[:, k:k + 1], None, ALU.is_equal)
                    tt("dve", pr, oh, posf, ALU.mult)
                    S.add("dve", lambda e, pk=pk, pr=pr, k=k: e.reduce_sum(out=pk[:, k:k + 1], in_=pr, axis=AX.X), reads=[pr], writes=[pk[:, k:k + 1]])
                    ts("dve", vk[:, k:k + 1], pk[:, k:k + 1], float(CAP), None, ALU.is_lt)
                    stt("dve", gate_all[:, t, k:k + 1], ex4[:, k:k + 1], nmx[:, 1:2], vk[:, k:k + 1], ALU.mult, ALU.mult)
                    if k == 0:
                        ts("dve", gm_d, oh, gate_all[:, t, 0:1], None, ALU.mult)
                    else:
                        stt("dve", gm_d, oh, gate_all[:, t, k:k + 1], gm_d, ALU.mult, ALU.add)
                    stt("dve", idxf[:, k:k + 1], ef[:, k:k + 1], float(CAP), pk[:, k:k + 1], ALU.mult, ALU.add)
                inv = A.alloc([128, 4])
                ts("dve", inv, vk, -1.0, 1.0, ALU.mult, ALU.add)
                tt("dve", idxf[:, 0:4], idxf[:, 0:4], vk, ALU.mult)
                stt("dve", idxf[:, 4:8], inv, tz[:, 0:1], idxf[:, 0:4], ALU.mult, ALU.add)
                stt("dve", idxf[:, 0:4], inv, float(ZROW), idxf[:, 0:4], ALU.mult, ALU.add)
                cp("dve", idx_all[:, t, 0:4], idxf[:, 4:8])
                cp("dve", idx_all[:, t, 4:8], idxf[:, 0:4])
                if debug:
                    dma("sp", dbg["idx"][t * 128:(t + 1) * 128, :], idx_all[:, t, :], "dbg")
                    dma("sp", dbg["gate"][t * 128:(t + 1) * 128, :], gate_all[:, t, :], "dbg")
                pg_ = lbank()
                tr(pg_[0:NE, 0:128], gm_d)
                cp("dve", gmT_all[:, t, :], pg_[0:NE, 0:128])
                for k in range(0 if NOSC else 4):
                    S.add("pool", lambda e, t=t, k=k, h2b=h2b: e.indirect_dma_start(
                        out=XS, out_offset=bass.IndirectOffsetOnAxis(ap=idx_all[:, t, k:k + 1], axis=0),
                        in_=h2b, in_offset=None),
                        reads=[h2b, idx_all[:, t, k:k + 1]], writes=[XS], dkey="xs")
                for t_ in (xt, x1, ss, h2b, h2T, lg, mx, mi, ef, nmx, ex4, posf, gm_d, idxf, oh, pr, pk, vk, inv):
                    A.release(t_)
            for t_ in (mTb, g1bc, A2bc, B2bc):
                A.release(t_)

        for t_ in (kT_all, V_all, kg_bc, qg_bc, ag_bc, gg_bc, wsp_b, bsp, wr_sb, br_sb, maskb):
            A.release(t_)
        for t_ in wring:
            A.release(t_)

        if upto < 3:
            return
        zt = A.alloc([128, D])
        memset("dve", zt, 0.0)
        dma("sp", YB[ZROW:ZROW + 128, :], zt, "ybz")
        A.release(zt)
        bg_sb = A.alloc([128, NE, 16])
        bu_sb = A.alloc([128, NE, 16])
        dma("sp", bg_sb, bgT, "misc")
        dma("sp", bu_sb, buT, "misc")
        ts("dve", bu_sb, bu_sb, 1.0, None, ALU.add)
        ering = [A.alloc([128, 16, 512], BF16) for _ in range(4)]
        er_i = [0]

        def eload(src2d):
            slot = er_i[0] % len(ering)
            er_i[0] += 1
            w = ering[slot]
            dma("pool", w, src2d.rearrange("(kc p) n -> p kc n", p=128), "e%d" % slot)
            return w

        xtk = A.alloc([128, 4, D], BF16)
        xsT2 = [A.alloc([128, 16, CAP], BF16) for _ in range(2)]
        actT2 = [A.alloc([128, 16, CAP], BF16) for _ in range(2)]
        identb = A.alloc([128, 128], BF16)
        cp("dve", identb, ident)

        def prep(e_):
            xsT = xsT2[e_ % 2]
            dma("sp", xtk, XS[e_ * CAP:(e_ + 1) * CAP, :].rearrange("(t p) d -> p t d", p=128), "xtk")
            for kc in range(16):
                pb = bank(kc % 2)
                for tb in range(4):
                    mm(pb[:, tb * 128:(tb + 1) * 128], xtk[:, tb, kc * 128:(kc + 1) * 128], identb)
                evac(xsT[:, kc, :], pb)

        def gate_up(e_):
            xsT = xsT2[e_ % 2]
            actT = actT2[e_ % 2]
            for mg in range(4):
                wg_ = eload(w_gate[e_, :, mg * 512:(mg + 1) * 512])
                wu_ = eload(w_up[e_, :, mg * 512:(mg + 1) * 512])
                for m4 in range(4):
                    mc = mg * 4 + m4
                    pg = bank(2 + (mc % 2))
                    pu = bank(4 + (mc % 2))
                    for kc in range(16):
                        mm(pg, wg_[:, kc, m4 * 128:(m4 + 1) * 128], xsT[:, kc, :], start=(kc == 0), stop=(kc == 15))
                    for kc in range(16):
                        mm(pu, wu_[:, kc, m4 * 128:(m4 + 1) * 128], xsT[:, kc, :], start=(kc == 0), stop=(kc == 15))
                    gt = A.alloc([128, CAP])
                    sg = A.alloc([128, CAP])
                    up = A.alloc([128, CAP])
                    ts("dve", gt, pg, bg_sb[:, e_, mc:mc + 1], c7[:, 0:1], ALU.add, ALU.min)
                    act(sg, gt, AF.Sigmoid, scale=1.702)
                    ts("dve", up, pu, bu_sb[:, e_, mc:mc + 1], cm6[:, 0:1], ALU.add, ALU.max)
                    tt("dve", gt, gt, sg, ALU.mult)
                    stt("dve", actT[:, mc, :], up, 8.0, gt, ALU.min, ALU.mult)
                    for t_ in (gt, sg, up):
                        A.release(t_)

        def down(e_):
            actT = actT2[e_ % 2]
            for n in range(4):
                wd_ = eload(w_down[e_, :, n * 512:(n + 1) * 512])
                for tb in range(4):
                    py = bank(6 + (tb % 2))
                    for fc in range(16):
                        mm(py, actT[:, fc, tb * 128:(tb + 1) * 128], wd_[:, fc, :], start=(fc == 0), stop=(fc == 15))
                    yo = A.alloc([128, 512])
                    cp("act", yo, py)
                    r0 = e_ * CAP + tb * 128
                    dma("sp", YB[r0:r0 + 128, n * 512:(n + 1) * 512], yo, "yb")
                    A.release(yo)

        prep(0)
        for e_ in range(NE):
            gate_up(e_)
            if e_ + 1 < NE:
                prep(e_ + 1)
            down(e_)
        for t_ in (bg_sb, bu_sb, xtk, xsT2[0], xsT2[1], actT2[0], actT2[1], identb):
            A.release(t_)
        for t_ in ering:
            A.release(t_)

        if upto < 4:
            return
        bd_sb = A.alloc([NE, D])
        dma("sp", bd_sb, b_down, "misc")
        nf_bc = A.alloc([128, D])
        bc_load(nf_bc, norm_f[0:1, :])
        g2 = [A.alloc([128, D]) for _ in range(2)]
        bc_load(g2[0], MODS[0:1, 5, :])
        bc_load(g2[1], MODS[1:2, 5, :])
        for t in range(NT):
            g = 0 if t < 4 else 1
            f = A.alloc([128, D])
            for n in range(4):
                mm(PS[:, n, :], gmT_all[:, t, :], bd_sb[:, n * 512:(n + 1) * 512])
            for k in range(4):
                r = A.alloc([128, D])
                S.add("pool", lambda e, t=t, k=k, r=r: e.indirect_dma_start(
                    out=r, out_offset=None, in_=YB, in_offset=bass.IndirectOffsetOnAxis(ap=idx_all[:, t, 4 + k:5 + k], axis=0)),
                    reads=[YB, idx_all[:, t, 4 + k:5 + k]], writes=[r], dkey="yg%d" % (k % 2))
                if k == 0:
                    for n in range(4):
                        stt("dve", f[:, n * 512:(n + 1) * 512], r[:, n * 512:(n + 1) * 512], gate_all[:, t, 0:1], PS[:, n, :], ALU.mult, ALU.add)
                else:
                    stt("dve", f, r, gate_all[:, t, k:k + 1], f, ALU.mult, ALU.add)
                A.release(r)
            if debug:
                dma("sp", dbg["f"][t * 128:(t + 1) * 128, :], f, "dbg")
            x1 = A.alloc([128, D])
            dma("sp", x1, X1[t * 128:(t + 1) * 128, :], "x1r")
            tt("dve", f, f, g2[g], ALU.mult)
            tt("pool", f, f, x1, ALU.add)
            ss = A.alloc([128, 2])
            act(x1, f, AF.Square, accum=ss[:, 0:1])
            rstd_from_ss(ss[:, 1:2], ss[:, 0:1], D)
            stt("dve", x1, f, ss[:, 1:2], nf_bc, ALU.mult, ALU.mult)
            dma("sp", y_own[t * 128:(t + 1) * 128, :], x1, "yout")
            for t_ in (f, x1, ss):
                A.release(t_)


    emit()
    S.finish()
    es.close()
    build_nc.stats = dict(S.stats, sb_peak=A.peak)
    return nc


def _rope_tables():
    n = 4096
    row = np.repeat(np.arange(n // 64, dtype=np.float32), 64)
    col = np.tile(np.arange(64, dtype=np.float32), n // 64)
    inv = (np.float32(10000.0) ** (-np.arange(32, dtype=np.float32) / np.float32(32))).astype(np.float32)
    ar = row[:, None] * inv[None, :]
    ac = col[:, None] * inv[None, :]
    cr, sr, cc, sc_ = np.cos(ar), np.sin(ar), np.cos(ac), np.sin(ac)
    cos4 = np.concatenate([cr, cr, cc, cc], 1).astype(np.float32)
    sin4 = np.concatenate([-sr, sr, -sc_, sc_], 1).astype(np.float32)
    return cos4, sin4


def core_inputs(I, c, shared):
    sb, qt = c // 4, c % 4
    m = dict(shared)
    m["x_own"] = np.ascontiguousarray(np.concatenate(
        [I["x_prompt"][2 * c].reshape(256, D), I["x_prompt"][2 * c + 1].reshape(256, D),
         I["x_sample"][sb, qt * 1024:(qt + 1) * 1024]], 0))
    m["x_kv"] = np.ascontiguousarray(I["x_sample"][sb])
    m["ck"] = np.ascontiguousarray(I["cache_k"][sb, 0].reshape(256, 256))
    m["cv"] = np.ascontiguousarray(I["cache_v"][sb, 0].reshape(256, 256))
    cvecs = np.stack([I["c_ctx"], I["c"][sb]], 1)
    m["cT"] = np.ascontiguousarray(cvecs.reshape(16, 128, 2).transpose(1, 0, 2))
    m["cos_q"] = np.ascontiguousarray(shared["cos_k"][qt * 1024:(qt + 1) * 1024])
    m["sin_q"] = np.ascontiguousarray(shared["sin_k"][qt * 1024:(qt + 1) * 1024])
    return m


def shared_inputs(I):
    cos4, sin4 = _rope_tables()
    s = {
        "w_mod": I["w_mod"][0], "b_mod": I["b_mod"][0].reshape(1, -1), "norm1": I["norm1"][0].reshape(1, -1),
        "norm2": I["norm2"][0].reshape(1, -1), "norm_f": I["norm_f"].reshape(1, -1), "w_in": I["w_in"][0],
        "w_out": I["w_out"][0], "w_router": I["w_router"][0], "b_router": I["b_router"][0].reshape(1, -1),
        "q_gain": I["q_gain"][0].reshape(1, -1), "k_gain": I["k_gain"][0].reshape(1, -1),
        "w_spT": np.ascontiguousarray(I["w_sp"][0].transpose(2, 0, 1)),
        "b_spT": np.ascontiguousarray(I["b_sp"][0].T),
        "ag": I["attn_out_gain"][0].reshape(1, -1), "gg": I["gmlp_out_gain"][0].reshape(1, -1),
        "w_gate": I["w_gate"][0], "w_up": I["w_up"][0], "w_down": I["w_down"][0],
        "bgT": np.ascontiguousarray(I["b_gate"][0].reshape(NE, 16, 128).transpose(2, 0, 1)),
        "buT": np.ascontiguousarray(I["b_up"][0].reshape(NE, 16, 128).transpose(2, 0, 1)),
        "b_down": I["b_down"][0], "cos_k": cos4, "sin_k": sin4,
    }
    return {k: np.ascontiguousarray(np.asarray(v, dtype=np.float32)) for k, v in s.items()}


def kernel(**inputs):
    I = {k: np.asarray(v) for k, v in inputs.items()}
    shared = shared_inputs(I)
    in_maps = [core_inputs(I, c, shared) for c in range(8)]
    nc = build_nc()
    res = run_bass_kernel_spmd(nc, in_maps, core_ids=list(range(8)))
    y_prompt = np.zeros((16, 256, D), np.float32)
    y_sample = np.zeros((2, 4096, D), np.float32)
    nck = np.zeros((16, 1, 256, 2, 128), np.float32)
    ncv = np.zeros((16, 1, 256, 2, 128), np.float32)
    for c in range(8):
        r = res.results[c]
        sb, qt = c // 4, c % 4
        y = r["y_own"]
        y_prompt[2 * c] = y[0:256]
        y_prompt[2 * c + 1] = y[256:512]
        y_sample[sb, qt * 1024:(qt + 1) * 1024] = y[512:]
        nck[2 * c, 0] = r["nk"][0:256].reshape(256, 2, 128)
        nck[2 * c + 1, 0] = r["nk"][256:512].reshape(256, 2, 128)
        ncv[2 * c, 0] = r["nv"][0:256].reshape(256, 2, 128)
        ncv[2 * c + 1, 0] = r["nv"][256:512].reshape(256, 2, 128)
    return (y_prompt, y_sample, nck, ncv)
```
